# Optimizing a Trainium2 kernel written in Bass

```python
import jax, jax.numpy as jnp
from jax import lax
import numpy as np

D_MODEL = 1024
BATCH = 16
SEQ = 4096
DEPTH = 1

GLA_HEADS = 4
GLA_DK = 64
GLA_DV = 128
GLA_KEY_W = GLA_HEADS * GLA_DK
GLA_VAL_W = GLA_HEADS * GLA_DV
GLA_GATE_RANK = 16
GLA_GATE_NORMALIZER = 16.0
GLA_CHUNK = 64
POOL_GROUPS = 4
POOL_WINDOWS = (2, 4, 8, 16)
POOL_W = D_MODEL // 2
POOL_GW = POOL_W // POOL_GROUPS
IN_WIDTHS = (GLA_KEY_W, GLA_KEY_W, GLA_VAL_W, GLA_VAL_W, GLA_GATE_RANK, POOL_W, D_MODEL, D_MODEL)
D_IN = sum(IN_WIDTHS)
N_GROUPS = 4
EXPERTS_PER_GROUP = 8
N_EXPERTS = N_GROUPS * EXPERTS_PER_GROUP
TOP_K_IN_GROUP = 2
D_EXPERT = 256
MOE_BLOCK = 128
NORM_EPS = 1e-6

kernel_name = "hybrid_gla_pool_hmoe_block"


def rmsnorm(x, g):
    xf = x.astype(jnp.float32)
    y = xf * lax.rsqrt(jnp.mean(xf * xf, axis=-1, keepdims=True) + NORM_EPS)
    return (y * g.astype(jnp.float32)).astype(x.dtype)


def gla_mix(q, k, v, log_a):
    B, S, _ = q.shape
    C = GLA_CHUNK
    N = S // C

    def split(t, d):
        return t.astype(jnp.float32).reshape(B, N, C, GLA_HEADS, d).transpose(0, 3, 1, 2, 4)

    qc, kc, gc = split(q, GLA_DK), split(k, GLA_DK), split(log_a, GLA_DK)
    vc = split(v, GLA_DV)
    b = jnp.cumsum(gc, axis=3)
    b_last = b[:, :, :, -1:, :]
    q_dec = qc * jnp.exp(b) * (GLA_DK ** -0.5)
    k_dec = kc * jnp.exp(-b)
    causal = jnp.tril(jnp.ones((C, C), dtype=bool))
    scores = jnp.where(causal, jnp.einsum('bhntd,bhnsd->bhnts', q_dec, k_dec), 0.0)
    o_intra = jnp.einsum('bhnts,bhnsv->bhntv', scores, vc)
    k_end = kc * jnp.exp(b_last - b)
    inc = jnp.einsum('bhnsd,bhnsv->bhndv', k_end, vc)
    decay = jnp.exp(b_last[:, :, :, 0, :])

    def step(state, inp):
        dec, u = inp
        return dec[..., None] * state + u, state

    state0 = jnp.zeros((B, GLA_HEADS, GLA_DK, GLA_DV), jnp.float32)
    _, s_prev = lax.scan(step, state0, (jnp.moveaxis(decay, 2, 0), jnp.moveaxis(inc, 2, 0)))
    s_prev = jnp.moveaxis(s_prev, 0, 2)
    o = o_intra + jnp.einsum('bhntd,bhndv->bhntv', q_dec, s_prev)
    return o.transpose(0, 2, 3, 1, 4).reshape(B, S, GLA_VAL_W)


def head_rmsnorm(o, g):
    B, S, _ = o.shape
    oh = o.reshape(B, S, GLA_HEADS, GLA_DV)
    oh = oh * lax.rsqrt(jnp.mean(oh * oh, axis=-1, keepdims=True) + NORM_EPS)
    return oh.reshape(B, S, GLA_VAL_W) * g.astype(jnp.float32)


def pool_mix(u, pool_w, pool_scale):
    B, S, _ = u.shape
    uf = u.astype(jnp.float32)
    ug = uf.reshape(B, S, POOL_GROUPS, POOL_GW)
    cs = jnp.cumsum(uf, axis=1).reshape(B, S, POOL_GROUPS, POOL_GW)
    pos = jnp.arange(S, dtype=jnp.float32)
    pooled = []
    for gi, w in enumerate(POOL_WINDOWS):
        c = cs[:, :, gi]
        shifted = jnp.pad(c[:, :S - w], ((0, 0), (w, 0), (0, 0)))
        cnt = jnp.minimum(pos + 1.0, float(w))[None, :, None]
        pooled.append((c - shifted) / cnt)
    pooled = jnp.stack(pooled, axis=2)
    mixed = jnp.einsum('bsgc,gcd->bsgd', pooled - ug, pool_w.astype(jnp.float32))
    return (mixed.reshape(B, S, POOL_W) * pool_scale.astype(jnp.float32)).astype(u.dtype)


def hier_moe(h, w_rg, b_rg, w_re, b_re, w_gate, w_up, w_down):
    B, S, D = h.shape
    T = B * S
    ht = h.reshape(T, D)
    g_logits = (ht @ w_rg).astype(jnp.float32) + b_rg.astype(jnp.float32)
    g_p, g_idx = lax.top_k(jax.nn.softmax(g_logits, axis=-1), 1)
    e_all = jnp.einsum('td,gde->tge', ht, w_re).astype(jnp.float32) + b_re.astype(jnp.float32)
    e_logits = e_all[jnp.arange(T), g_idx[:, 0]]
    e_top, e_idx = lax.top_k(e_logits, TOP_K_IN_GROUP)
    gate = g_p * jax.nn.softmax(e_top, axis=-1)
    A = T * TOP_K_IN_GROUP
    weights = gate.reshape(A)
    expert_ids = (g_idx * EXPERTS_PER_GROUP + e_idx).reshape(A).astype(jnp.int32)
    token_ids = jnp.repeat(jnp.arange(T, dtype=jnp.int32), TOP_K_IN_GROUP)

    n_blocks = -(-(A + N_EXPERTS * (MOE_BLOCK - 1)) // MOE_BLOCK)
    R = n_blocks * MOE_BLOCK
    order = jnp.argsort(expert_ids)
    sorted_e = expert_ids[order]
    counts = jnp.zeros((N_EXPERTS,), jnp.int32).at[expert_ids].add(1)
    padded = ((counts + MOE_BLOCK - 1) // MOE_BLOCK) * MOE_BLOCK
    starts = jnp.cumsum(counts) - counts
    pends = jnp.cumsum(padded)
    pstarts = pends - padded
    dest = pstarts[sorted_e] + (jnp.arange(A, dtype=jnp.int32) - starts[sorted_e])
    row_token = jnp.full((R,), T, jnp.int32).at[dest].set(token_ids[order])
    row_w = jnp.zeros((R,), jnp.float32).at[dest].set(weights[order])
    block_e = jnp.minimum(jnp.searchsorted(pends, jnp.arange(n_blocks, dtype=jnp.int32) * MOE_BLOCK,
                                           side='right'), N_EXPERTS - 1).astype(jnp.int32)
    h_pad = jnp.concatenate([ht, jnp.zeros((1, D), ht.dtype)], axis=0)
    x_blocks = h_pad[row_token].reshape(n_blocks, MOE_BLOCK, D)

    def expert_block(args):
        xb, e = args
        return (jax.nn.silu(xb @ w_gate[e]) * (xb @ w_up[e])) @ w_down[e]

    y_rows = lax.map(expert_block, (x_blocks, block_e)).reshape(R, D)
    y_rows = y_rows * row_w[:, None].astype(y_rows.dtype)
    out = jnp.zeros((T + 1, D), y_rows.dtype).at[row_token].add(y_rows)[:T]
    return out.reshape(B, S, D).astype(h.dtype)


def setup_inputs(seed: int = 0) -> dict:
    key = jax.random.key(seed)
    ks = jax.random.split(key, 24)
    L, D = DEPTH, D_MODEL
    nrm = lambda k, shape, s: jax.random.normal(k, shape, jnp.float32) * s
    return {
        "x": nrm(ks[0], (BATCH, SEQ, D), 1.0),
        "norm1_g": 1.0 + nrm(ks[1], (L, D), 0.05),
        "w_in": nrm(ks[2], (L, D, D_IN), D ** -0.5),
        "w_alpha_up": nrm(ks[3], (L, GLA_GATE_RANK, GLA_KEY_W), GLA_GATE_RANK ** -0.5),
        "b_alpha": nrm(ks[4], (L, GLA_KEY_W), 0.1),
        "gla_norm_g": 1.0 + nrm(ks[5], (L, GLA_VAL_W), 0.05),
        "w_gla_branch": nrm(ks[6], (L, GLA_VAL_W, D), GLA_VAL_W ** -0.5),
        "pool_w": nrm(ks[7], (L, POOL_GROUPS, POOL_GW, POOL_GW), POOL_GW ** -0.5),
        "pool_scale": 1.0 + nrm(ks[8], (L, POOL_W), 0.05),
        "w_pool_branch": nrm(ks[9], (L, POOL_W, D), POOL_W ** -0.5),
        "w_out": nrm(ks[10], (L, D, D), D ** -0.5),
        "norm2_g": 1.0 + nrm(ks[11], (L, D), 0.05),
        "w_router_group": nrm(ks[12], (L, D, N_GROUPS), D ** -0.5),
        "b_router_group": nrm(ks[13], (L, N_GROUPS), 0.01),
        "w_router_expert": nrm(ks[14], (L, N_GROUPS, D, EXPERTS_PER_GROUP), D ** -0.5),
        "b_router_expert": nrm(ks[15], (L, N_GROUPS, EXPERTS_PER_GROUP), 0.01),
        "w_exp_gate": nrm(ks[16], (L, N_EXPERTS, D, D_EXPERT), D ** -0.5),
        "w_exp_up": nrm(ks[17], (L, N_EXPERTS, D, D_EXPERT), D ** -0.5),
        "w_exp_down": nrm(ks[18], (L, N_EXPERTS, D_EXPERT, D), D_EXPERT ** -0.5),
        "norm_f_g": 1.0 + nrm(ks[19], (D,), 0.05),
    }


def reference(x, norm1_g, w_in, w_alpha_up, b_alpha, gla_norm_g, w_gla_branch, pool_w, pool_scale,
              w_pool_branch, w_out, norm2_g, w_router_group, b_router_group, w_router_expert,
              b_router_expert, w_exp_gate, w_exp_up, w_exp_down, norm_f_g):
    split_idx = [int(i) for i in np.cumsum(IN_WIDTHS)[:-1]]
    for l in range(DEPTH):
        h = rmsnorm(x, norm1_g[l])
        proj = h @ w_in[l]
        q, k, v, r, a_low, u_pool, gate_gla, gate_pool = jnp.split(proj, split_idx, axis=-1)
        log_a = jax.nn.log_sigmoid((a_low @ w_alpha_up[l] + b_alpha[l]).astype(jnp.float32)) / GLA_GATE_NORMALIZER
        o = gla_mix(q, k, v, log_a)
        o = (head_rmsnorm(o, gla_norm_g[l]) * jax.nn.silu(r.astype(jnp.float32))).astype(x.dtype)
        y_gla = o @ w_gla_branch[l]
        y_pool = pool_mix(u_pool, pool_w[l], pool_scale[l]) @ w_pool_branch[l]
        merged = jax.nn.sigmoid(gate_gla) * y_gla + jax.nn.sigmoid(gate_pool) * y_pool
        x = x + merged @ w_out[l]
        h2 = rmsnorm(x, norm2_g[l])
        x = x + hier_moe(h2, w_router_group[l], b_router_group[l], w_router_expert[l], b_router_expert[l],
                         w_exp_gate[l], w_exp_up[l], w_exp_down[l])
    return rmsnorm(x, norm_f_g)
```

```python
import contextlib
import numpy as np
import ml_dtypes
import concourse.bass as bass
import concourse.mybir as mybir
from concourse.bass_utils import run_bass_kernel_spmd

F32 = mybir.dt.float32
BF16 = mybir.dt.bfloat16
I32 = mybir.dt.int32
AF = mybir.ActivationFunctionType
ALU = mybir.AluOpType
AX = mybir.AxisListType

NCORES = 8
D = 1024
SEQ = 4096
TOK = 8192
NT = 64
TPS = 32
DIN = 4112
NE = 32
NWB = 96
NBLK = 2 * NWB
NROWS = NBLK * 128
EPS = 1e-6

ENGINES = ("sp", "act", "dve", "pool", "pe")
STRICT = {"act": True, "dve": True, "pool": True, "pe": False, "sp": False}


class Buf:
    __slots__ = ("name", "lw", "rd")

    def __init__(self, name):
        self.name = name
        self.lw = None
        self.rd = []


class Prog:
    def __init__(self, nc):
        self.nc = nc
        self.ops = {e: [] for e in ENGINES}
        self.clk = {e: {} for e in ENGINES}
        self.waited_own = {e: 0 for e in ENGINES}
        self.opclock = {}
        self.dma_cnt = {}
        self.dma_last = {}
        self.signal = set()
        self.max_ops = None
        self.nrec = 0

    def _over(self):
        self.nrec += 1
        return self.max_ops is not None and self.nrec > self.max_ops

    def _resolve(self, eng, is_dma, reads, writes, extra):
        deps = set(extra)
        for b in reads:
            if b.lw is not None:
                deps.add(b.lw)
        for b in writes:
            if b.lw is not None:
                deps.add(b.lw)
            deps.update(b.rd)
        clk = self.clk[eng]
        waits = []
        for tok in sorted(deps, key=lambda t: (t[0], t[1], -t[2])):
            k = (tok[0], tok[1])
            if tok[0] == "c" and tok[1] == eng:
                if not (is_dma or STRICT[eng]):
                    continue
                if self.waited_own[eng] >= tok[2]:
                    continue
                self.waited_own[eng] = tok[2]
                waits.append(tok)
                self.signal.add(tok)
                continue
            if clk.get(k, 0) >= tok[2]:
                continue
            waits.append(tok)
            if tok[0] == "c":
                self.signal.add(tok)
            oc = self.opclock.get(tok)
            if oc:
                for kk, vv in oc.items():
                    if kk == ("c", eng):
                        continue
                    if clk.get(kk, 0) < vv:
                        clk[kk] = vv
            if clk.get(k, 0) < tok[2]:
                clk[k] = tok[2]
        return waits

    def _finish(self, tok, reads, writes):
        for b in reads:
            b.rd.append(tok)
        for b in writes:
            b.lw = tok
            b.rd = []

    def op(self, eng, fn, reads=(), writes=(), extra=()):
        if self._over():
            return None
        waits = self._resolve(eng, False, reads, writes, extra)
        lst = self.ops[eng]
        idx = len(lst) + 1
        tok = ("c", eng, idx)
        lst.append(dict(fn=fn, waits=waits, dma=None))
        self.clk[eng][("c", eng)] = idx
        self.opclock[tok] = dict(self.clk[eng])
        self._finish(tok, reads, writes)
        return tok

    def dma(self, eng, fn, reads=(), writes=(), sem=None, extra=()):
        if self._over():
            return None
        ex = list(extra)
        if sem in self.dma_last:
            ex.append(self.dma_last[sem])
        waits = self._resolve(eng, True, reads, writes, ex)
        cnt = self.dma_cnt.get(sem, 0) + 16
        self.dma_cnt[sem] = cnt
        tok = ("d", sem, cnt)
        self.dma_last[sem] = tok
        lst = self.ops[eng]
        idx = len(lst) + 1
        lst.append(dict(fn=fn, waits=waits, dma=sem))
        self.clk[eng][("c", eng)] = idx
        self.opclock[tok] = dict(self.clk[eng])
        self._finish(tok, reads, writes)
        return tok

    def wait_only(self, eng, reads=(), writes=(), extra=()):
        waits = self._resolve(eng, True, reads, writes, extra)
        lst = self.ops[eng]
        lst.append(dict(fn=None, waits=waits, dma=None))
        self.clk[eng][("c", eng)] = len(lst)

    def barrier(self):
        toks = []
        for e in ENGINES:
            for i in range(len(self.ops[e]), 0, -1):
                o = self.ops[e][i - 1]
                if o["fn"] is not None and o["dma"] is None:
                    toks.append(("c", e, i))
                    break
        toks.extend(self.dma_last.values())
        for e in ENGINES:
            self.wait_only(e, extra=toks)

    def emit(self, stack):
        nc = self.nc
        esem = {e: stack.enter_context(nc.semaphore("es_" + e)) for e in ENGINES}
        dsem = {k: stack.enter_context(nc.semaphore("ds_%d" % i)) for i, k in enumerate(self.dma_cnt)}
        signum = {}
        for e in ENGINES:
            c = 0
            for i, o in enumerate(self.ops[e]):
                tok = ("c", e, i + 1)
                if tok in self.signal:
                    assert o["fn"] is not None and o["dma"] is None, (e, i)
                    c += 1
                    signum[tok] = c

        def run(e, engobj):
            for i, o in enumerate(self.ops[e]):
                for w in o["waits"]:
                    if w[0] == "c":
                        engobj.wait_ge(esem[w[1]], signum[w])
                    else:
                        engobj.wait_ge(dsem[w[1]], w[2])
                if o["fn"] is None:
                    continue
                inst = o["fn"](engobj)
                if o["dma"] is not None:
                    inst.then_inc(dsem[o["dma"]], 16)
                elif ("c", e, i + 1) in signum:
                    inst.then_inc(esem[e], 1)

        with nc.Block() as block:
            @block.sync
            def _(x):
                run("sp", x)

            @block.scalar
            def _(x):
                run("act", x)

            @block.vector
            def _(x):
                run("dve", x)

            @block.gpsimd
            def _(x):
                run("pool", x)

            @block.tensor
            def _(x):
                run("pe", x)


def MM(out, lhsT, rhs, start=True, stop=True):
    return lambda e: e.matmul(out, lhsT=lhsT, rhs=rhs, start=start, stop=stop)


def TR(out, in_, ident):
    return lambda e: e.transpose(out=out, in_=in_, identity=ident)


def TT(out, in0, in1, op):
    return lambda e: e.tensor_tensor(out=out, in0=in0, in1=in1, op=op)


def TS(out, in0, s1, op0, s2=None, op1=None):
    if op1 is None:
        return lambda e: e.tensor_scalar(out=out, in0=in0, scalar1=s1, scalar2=None, op0=op0)
    return lambda e: e.tensor_scalar(out=out, in0=in0, scalar1=s1, scalar2=s2, op0=op0, op1=op1)


def STT(out, in0, scalar, in1, op0, op1):
    return lambda e: e.scalar_tensor_tensor(out=out, in0=in0, scalar=scalar, in1=in1, op0=op0, op1=op1)


def ACT(out, in_, func, **kw):
    return lambda e: e.activation(out=out, in_=in_, func=func, **kw)


def CP(out, in_):
    return lambda e: e.tensor_copy(out=out, in_=in_)


def ACP(out, in_):
    return lambda e: e.copy(out=out, in_=in_)


def RED(out, in_, op):
    return lambda e: e.tensor_reduce(out=out, in_=in_, axis=AX.X, op=op)


def RCP(out, in_):
    return lambda e: e.reciprocal(out=out, in_=in_)


def MSET(ap, v):
    return lambda e: e.memset(ap, v)


def DMA(out, in_):
    return lambda e: e.dma_start(out=out, in_=in_)


def SCAT(out, idx, in_):
    return lambda e: e.indirect_dma_start(out=out, out_offset=bass.IndirectOffsetOnAxis(ap=idx, axis=0),
                                          in_=in_, in_offset=None)


def GATH(out, in_, idx):
    return lambda e: e.indirect_dma_start(out=out, out_offset=None, in_=in_,
                                          in_offset=bass.IndirectOffsetOnAxis(ap=idx, axis=0))


CF_TRI, CF_REV, CF_MASK, CF_THR, CF_BV, CF_PIDX, NCF = 0, 128, 256, 768, 832, 928, 929
CB_ID, CB_ONES, CB_LS, CB_A, CB_B, CB_A1, NCB = 0, 128, 256, 384, 896, 1408, 1920


def build_program(ntr=NT, stop=None, max_ops=None):
    nc = bass.Bass("TRN2", target_bir_lowering=False)

    def din(name, shape, dt=F32):
        return nc.dram_tensor(name, shape, dt, kind="ExternalInput").ap()

    def dscr(name, shape, dt):
        return nc.dram_tensor(name, shape, dt, kind="Internal").ap()

    x_d = din("x", [TOK, D])
    w_in_d = din("w_in", [D, DIN])
    wa_d = din("wa", [17, 256])
    cols_d = din("cols", [128, 24])
    wgb_d = din("wgb", [512, D])
    wpb_d = din("wpb", [512, D])
    wout_d = din("wout", [D, D])
    poolw_d = din("poolw", [4, 128, 128])
    wr_d = din("wr", [D, 36])
    br_d = din("br", [1, 36])
    weg_d = din("weg", [NE, D, 256])
    weu_d = din("weu", [NE, D, 256])
    wed_d = din("wed", [NE, 256, D])
    gf_d = din("gf", [D])
    g2_d = din("g2", [D])
    cf_d = din("cf", [128, NCF])
    cb_d = din("cb", [128, NCB], BF16)
    zeros_d = din("zeros", [128, 8192], BF16)
    out_d = nc.dram_tensor("out", [TOK, D], F32, kind="ExternalOutput").ap()

    x2_d = dscr("x2s", [TOK, D], F32)
    h2_d = dscr("h2s", [TOK, D], BF16)
    xs_d = dscr("xsort", [NROWS, D], BF16)
    y_d = dscr("ysort", [NROWS, D], F32)
    wall_d = dscr("wall", [NE * 128, 8 * 512 + 2 * 1024], BF16)

    top = contextlib.ExitStack()
    with top:
        p = Prog(nc)
        p.max_ops = max_ops

        class T:
            def __init__(self, stack, name, shape, dt, psum=False):
                if psum:
                    self.t = stack.enter_context(nc.psum_tensor("t_" + name, shape, dt))
                else:
                    self.t = stack.enter_context(nc.sbuf_tensor("t_" + name, shape, dt))
                self.b = Buf(name)

        cf = T(top, "cf", [128, NCF], F32)
        cb = T(top, "cb", [128, NCB], BF16)
        cols = T(top, "cols", [128, 24], F32)
        gf = T(top, "gf", [128, D], F32)
        LG = T(top, "LG", [128, NT, 36], F32)
        Wall = T(top, "Wall", [128, NT, 2], F32)
        desti = T(top, "desti", [128, 2, NT], I32)
        widxi = T(top, "widxi", [128, NWB], I32)

        p.dma("sp", DMA(cf.t[:], cf_d), writes=[cf.b], sem="c0")
        p.dma("sp", DMA(cb.t[:], cb_d), writes=[cb.b], sem="c1")
        p.dma("sp", DMA(cols.t[:], cols_d), writes=[cols.b], sem="c2")
        p.dma("act", DMA(gf.t[:], gf_d.partition_broadcast(128)), writes=[gf.b], sem="c3")

        ident = cb.t[:, CB_ID:CB_ID + 128]
        ones_bf = cb.t[:, CB_ONES:CB_ONES + 128]
        lstrict = cb.t[:, CB_LS:CB_LS + 128]
        poolA = cb.t[:, CB_A:CB_A + 512].rearrange("p (g t) -> p g t", g=4)
        poolB = cb.t[:, CB_B:CB_B + 512].rearrange("p (g t) -> p g t", g=4)
        poolA1 = cb.t[:, CB_A1:CB_A1 + 512].rearrange("p (g t) -> p g t", g=4)
        tri_incl = cf.t[:, CF_TRI:CF_TRI + 128]
        tri_rev = cf.t[:, CF_REV:CF_REV + 128]
        cmask = cf.t[:, CF_MASK:CF_MASK + 512]
        thr = cf.t[:, CF_THR:CF_THR + 64]
        bvals = cf.t[:, CF_BV:CF_BV + NWB]
        pidx = cf.t[:, CF_PIDX:CF_PIDX + 1]
        g1col = cols.t[:, 0:8]
        g2col = cols.t[:, 8:16]
        glncol = cols.t[:, 16:20]
        psccol = cols.t[:, 20:24]

        x_t = x_d.rearrange("(n p) d -> n p d", p=128)
        out_t = out_d.rearrange("(n p) d -> n p d", p=128)
        x2_t = x2_d.rearrange("(n p) d -> n p d", p=128)
        h2_t = h2_d.rearrange("(n p) d -> n p d", p=128)
        xs_t = xs_d.rearrange("(n p) d -> n p d", p=128)
        y_t = y_d.rearrange("(n p) d -> n p d", p=128)
        wall_t = wall_d.rearrange("(e p) f -> e p f", p=128)
        b_x2 = [Buf("x2d%d" % i) for i in range(NT)]
        b_h2 = [Buf("h2d%d" % i) for i in range(NT)]
        b_sc = [Buf("scd%d" % i) for i in range(2 * NT)]
        b_y = [Buf("yd%d" % i) for i in range(NBLK)]
        b_wgu = [Buf("wgud%d" % i) for i in range(NE)]
        b_wd = [Buf("wdd%d" % i) for i in range(NE)]
        b_out = [Buf("outd%d" % i) for i in range(NT)]
        NXZ = NROWS * D // (128 * 8192)
        b_xz = [Buf("xz%d" % i) for i in range(NXZ)]
        xs_flat = xs_d.rearrange("(p r) d -> p (r d)", p=128)

        s1 = contextlib.ExitStack()
        w_in = T(s1, "w_in", [128, 8, DIN], BF16)
        wgb = T(s1, "wgbb", [128, 4, D], BF16)
        wpb = T(s1, "wpbb", [128, 4, D], BF16)
        wout = T(s1, "woutb", [128, 8, D], BF16)
        poolw = T(s1, "poolwb", [128, 4, 128], BF16)
        wr = T(s1, "wrb", [128, 8, 36], BF16)
        brb = T(s1, "brb", [1, 36], BF16)
        wa = T(s1, "wa", [17, 256], F32)
        g2b = T(s1, "g2b", [128, D], F32)

        xs = [T(s1, "xs%d" % i, [128, D], F32) for i in range(4)]
        stt_ = [T(s1, "st%d" % i, [128, 16], F32) for i in range(2)]
        h_bf = T(s1, "h_bf", [128, D], BF16)
        hT = T(s1, "hT", [128, 8, 128], BF16)
        qk_tok = [T(s1, "qk_tok%d" % i, [128, 512], BF16) for i in range(2)]
        qkT = [T(s1, "qkT%d" % i, [128, 4, 128], BF16) for i in range(2)]
        zl = [T(s1, "zl%d" % i, [32, 128], F32) for i in range(2)]
        v_bf = [T(s1, "v_bf%d" % i, [128, 512], BF16) for i in range(2)]
        u_bf = [T(s1, "u_bf%d" % i, [128, 512], BF16) for i in range(4)]
        silu_r = [T(s1, "silu_r%d" % i, [128, 512], F32) for i in range(2)]
        r_sb = T(s1, "r_sb", [128, 512], F32)
        sgg = [T(s1, "sgg%d" % i, [128, D], F32) for i in range(2)]
        sgp = [T(s1, "sgp%d" % i, [128, D], F32) for i in range(2)]
        lsp = T(s1, "lsp", [128, 256], F32)
        epos = T(s1, "epos", [128, 256], F32)
        eneg = T(s1, "eneg", [128, 256], F32)
        erev = T(s1, "erev", [128, 256], F32)
        dec = T(s1, "dec", [128, 2], F32)
        qz = T(s1, "qz", [128, 4, 128], BF16)
        kdT = T(s1, "kdT", [128, 2, 128], BF16)
        kend = T(s1, "kend", [128, 256], BF16)
        scT = T(s1, "scT", [128, 4, 128], BF16)
        S2 = [T(s1, "S%d" % i, [128, 256], F32) for i in range(2)]
        S_bf2 = [T(s1, "S_bf%d" % i, [128, 256], BF16) for i in range(2)]
        sq = T(s1, "sq", [128, 512], F32)
        hst = T(s1, "hst", [128, 12], F32)
        on_bf = T(s1, "on_bf", [128, 512], BF16)
        onT = T(s1, "onT", [128, 4, 128], BF16)
        pm = T(s1, "pm", [128, 4, 128], BF16)
        mix = T(s1, "mix", [128, 4, 128], BF16)
        mrg = T(s1, "mrg", [128, D], BF16)
        mT = T(s1, "mT", [128, 8, 128], BF16)
        x2s = T(s1, "x2s", [128, D], F32)
        h2b = T(s1, "h2b", [128, D], BF16)
        h2T = T(s1, "h2T", [128, 8, 128], BF16)

        pT = T(s1, "pT", [128, D], BF16, psum=True)
        ring = [T(s1, "ring%d" % i, [128, 512], F32, psum=True) for i in range(3)]
        CZ = T(s1, "CZ", [128, 512], F32, psum=True)
        b_cza, b_czb = Buf("cza"), Buf("czb")
        CR = T(s1, "CR", [128, 512], F32, psum=True)
        CS = T(s1, "CS", [128, 512], F32, psum=True)
        CO = T(s1, "CO", [128, 512], F32, psum=True)
        CS3 = CS.t[:, :].rearrange("p (h t) -> p h t", h=4)
        rstate = [0]

        def ring_next():
            r = ring[rstate[0] % 3]
            rstate[0] += 1
            return r

        stg_ring = [sgg[0], sgp[0], sgg[1], sgp[1]]
        p.dma("act", DMA(g2b.t[:], g2_d.partition_broadcast(128)), writes=[g2b.b], sem="c5")
        sstate = [0]

        def stg_next():
            r = stg_ring[sstate[0] % len(stg_ring)]
            sstate[0] += 1
            return r
        ceng = ["dve", "pool"]
        w_in_v = w_in_d.rearrange("(k p) n -> p k n", p=128)
        for cbk in range(33):
            c0 = cbk * 128
            wdt = 128 if cbk < 32 else 16
            sg_ = stg_next()
            st3 = sg_.t[:, :].rearrange("p (k n) -> p k n", k=8)
            p.dma("sp", DMA(st3[:, :, 0:wdt], w_in_v[:, :, c0:c0 + wdt]), writes=[sg_.b], sem="stg" + sg_.b.name)
            p.op("dve", TT(w_in.t[:, :, c0:c0 + wdt], st3[:, :, 0:wdt],
                                   g1col.unsqueeze(2).to_broadcast([128, 8, wdt]), ALU.mult),
                 reads=[sg_.b, cols.b], writes=[w_in.b])
        wgb_v = wgb_d.rearrange("(k p) n -> p k n", p=128)
        wpb_v = wpb_d.rearrange("(k p) n -> p k n", p=128)
        wout_v = wout_d.rearrange("(k p) n -> p k n", p=128)
        for k in range(4):
            sg_ = stg_next()
            p.dma("sp", DMA(sg_.t[:, :], wgb_v[:, k, :]), writes=[sg_.b], sem="stg" + sg_.b.name)
            p.op("act", ACT(wgb.t[:, k, :], sg_.t[:, :], AF.Copy, scale=glncol[:, k:k + 1]),
                 reads=[sg_.b, cols.b], writes=[wgb.b])
            sg_ = stg_next()
            p.dma("sp", DMA(sg_.t[:, :], wpb_v[:, k, :]), writes=[sg_.b], sem="stg" + sg_.b.name)
            p.op("act", ACT(wpb.t[:, k, :], sg_.t[:, :], AF.Copy, scale=psccol[:, k:k + 1]),
                 reads=[sg_.b, cols.b], writes=[wpb.b])
        for k in range(8):
            sg_ = stg_next()
            p.dma("sp", DMA(sg_.t[:, :], wout_v[:, k, :]), writes=[sg_.b], sem="stg" + sg_.b.name)
            if k % 2 == 0:
                p.op("dve", CP(wout.t[:, k, :], sg_.t[:, :]), reads=[sg_.b], writes=[wout.b])
            else:
                p.op("act", ACP(wout.t[:, k, :], sg_.t[:, :]), reads=[sg_.b], writes=[wout.b])
        sg_ = stg_next()
        stp = sg_.t[:, 0:512].rearrange("p (g d) -> p g d", g=4)
        p.dma("sp", DMA(stp, poolw_d.rearrange("g c d -> c g d")), writes=[sg_.b], sem="stg" + sg_.b.name)
        p.op("dve", CP(poolw.t[:], stp), reads=[sg_.b], writes=[poolw.b])
        sg_ = stg_next()
        str_ = sg_.t[:, 0:288].rearrange("p (k n) -> p k n", k=8)
        p.dma("sp", DMA(str_, wr_d.rearrange("(k p) n -> p k n", p=128)), writes=[sg_.b], sem="stg" + sg_.b.name)
        p.op("dve", CP(wr.t[:], str_), reads=[sg_.b], writes=[wr.b])
        sg_ = stg_next()
        p.dma("sp", DMA(sg_.t[0:1, 0:36], br_d), writes=[sg_.b], sem="stg" + sg_.b.name)
        p.op("dve", CP(brb.t[:], sg_.t[0:1, 0:36]), reads=[sg_.b], writes=[brb.b])
        p.dma("act", DMA(wa.t[:], wa_d), writes=[wa.b], sem="c4")
        for z_ in zl:
            p.op("pool", MSET(z_.t[:], 1.0), writes=[z_.b])
        p.op("pool", MSET(qz.t[:], 0.0), writes=[qz.b])

        def convert_dma(e, pc):
            if pc < 2:
                src = (weg_d if pc == 0 else weu_d)[e].rearrange("(k p) n -> p k n", p=128)
                dst = wall_t[e][:, 0:4096].rearrange("p (k n) -> p k n", k=8)[:, :, pc * 256:(pc + 1) * 256]
                p.dma("pool", DMA(dst, src), writes=[b_wgu[e]], sem="cv%d" % (pc))
            else:
                src = wed_d[e].rearrange("(c p) n -> p c n", p=128)
                dst = wall_t[e][:, 4096:6144].rearrange("p (c n) -> p c n", c=2)
                p.dma("pool", DMA(dst, src), writes=[b_wd[e]], sem="cv2")

        def gt(n):
            return (n % 2) * TPS + n // 2

        def xload(t):
            Xn = xs[t % 4]
            p.dma("sp", DMA(Xn.t[:], x_t[gt(t)]), writes=[Xn.b], sem="xl%d" % (t % 4))

        def proj_tok(t, R, c0, n, bufs):
            for kc in range(8):
                p.op("pe", MM(R.t[:, 0:n], hT.t[:, kc, :], w_in.t[:, kc, c0:c0 + n], kc == 0, kc == 7),
                     reads=[w_in.b, hT.b], writes=bufs)

        def x_pre(t):
            X, st = xs[t % 4], stt_[t % 2]
            p.op("act", ACT(h_bf.t[:], X.t[:], AF.Square, accum_out=st.t[:, 0:1]), reads=[X.b], writes=[h_bf.b, st.b])
            p.op("act", ACT(st.t[:, 1:2], st.t[:, 0:1], AF.Ln, scale=1.0 / D, bias=EPS), reads=[st.b], writes=[st.b])
            p.op("act", ACT(st.t[:, 2:3], st.t[:, 1:2], AF.Exp, scale=-0.5), reads=[st.b], writes=[st.b])
            p.op("act", ACT(h_bf.t[:], X.t[:], AF.Copy, scale=st.t[:, 2:3]), reads=[X.b, st.b], writes=[h_bf.b])

        def x_hT(t):
            for kc in range(8):
                p.op("pe", TR(pT.t[:, kc * 128:(kc + 1) * 128], h_bf.t[:, kc * 128:(kc + 1) * 128], ident),
                     reads=[h_bf.b, cb.b], writes=[pT.b])
            p.op("dve", CP(hT.t[:].rearrange("p k t -> p (k t)"), pT.t[:]), reads=[pT.b], writes=[hT.b])

        def x_g1(t):
            R = ring_next()
            proj_tok(t, R, 0, 512, [R.b])
            p.op("act", ACP(qk_tok[t % 2].t[:], R.t[:, :]), reads=[R.b], writes=[qk_tok[t % 2].b])

        def x_g1b(t):
            Q = qk_tok[t % 2]
            for c in range(4):
                p.op("pe", TR(pT.t[:, c * 128:(c + 1) * 128], Q.t[:, c * 128:(c + 1) * 128], ident),
                     reads=[Q.b, cb.b], writes=[pT.b])
            p.op("dve", CP(qkT[t % 2].t[:].rearrange("p c t -> p (c t)"), pT.t[:, 0:512]), reads=[pT.b],
                 writes=[qkT[t % 2].b])

        def x_g2(t):
            R = ring_next()
            for kc in range(8):
                p.op("pe", MM(R.t[0:16, 0:128], w_in.t[:, kc, 1536:1552], hT.t[:, kc, :], kc == 0, kc == 7),
                     reads=[w_in.b, hT.b], writes=[R.b])
            p.op("dve", CP(zl[t % 2].t[0:16, :], R.t[0:16, 0:128]), reads=[R.b], writes=[zl[t % 2].b])

        def x_g3(t):
            R = ring_next()
            proj_tok(t, R, 512, 512, [R.b])
            p.op("act", ACP(v_bf[t % 2].t[:], R.t[:, :]), reads=[R.b], writes=[v_bf[t % 2].b])

        def x_g5(t):
            R = ring_next()
            proj_tok(t, R, 1552, 512, [R.b])
            p.op("act", ACP(u_bf[t % 4].t[:], R.t[:, :]), reads=[R.b], writes=[u_bf[t % 4].b])

        def sig_evac(dst_ap, dst_buf, R):
            p.op("act", ACT(dst_ap, R.t[:, :], AF.Exp, scale=-1.0), reads=[R.b], writes=[dst_buf])
            p.op("act", ACT(dst_ap, dst_ap, AF.Ln, bias=1.0), reads=[dst_buf], writes=[dst_buf])
            p.op("act", ACT(dst_ap, dst_ap, AF.Exp, scale=-1.0), reads=[dst_buf], writes=[dst_buf])

        def x_gr(t):
            par = t % 2
            R = ring_next()
            proj_tok(t, R, 1024, 512, [R.b])
            p.op("dve", CP(r_sb.t[:], R.t[:, :]), reads=[R.b], writes=[r_sb.b])
            p.op("act", ACT(silu_r[par].t[:], r_sb.t[:], AF.Exp, scale=-1.0), reads=[r_sb.b],
                 writes=[silu_r[par].b])
            p.op("act", ACT(silu_r[par].t[:], silu_r[par].t[:], AF.Ln, bias=1.0), reads=[silu_r[par].b],
                 writes=[silu_r[par].b])
            p.op("act", ACT(silu_r[par].t[:], silu_r[par].t[:], AF.Exp, scale=-1.0), reads=[silu_r[par].b],
                 writes=[silu_r[par].b])
            p.op("pool", TT(silu_r[par].t[:], r_sb.t[:], silu_r[par].t[:], ALU.mult), reads=[r_sb.b, silu_r[par].b],
                 writes=[silu_r[par].b])

        def x_gate(which, hh):
            def f(t):
                dst = (sgg if which == 0 else sgp)[t % 2]
                c0 = 2064 if which == 0 else 3088
                R = ring_next()
                proj_tok(t, R, c0 + hh * 512, 512, [R.b])
                sig_evac(dst.t[:, hh * 512:(hh + 1) * 512], dst.b, R)
            return f
        x_gg0, x_gg1, x_gp0, x_gp1 = x_gate(0, 0), x_gate(0, 1), x_gate(1, 0), x_gate(1, 1)

        def y0(t):
            par = t % 2
            p.op("pe", MM(CZ.t[:, 0:256], zl[par].t[0:17, :], wa.t[0:17, :]), reads=[zl[par].b, wa.b], writes=[b_cza])
            p.op("act", ACT(lsp.t[:], CZ.t[:, 0:256], AF.Exp, scale=-1.0), reads=[b_cza], writes=[lsp.b])
            p.op("act", ACT(lsp.t[:], lsp.t[:], AF.Ln, bias=1.0), reads=[lsp.b], writes=[lsp.b])

        def y1(t):
            par = t % 2
            for c in range(2):
                p.op("pe", MM(CZ.t[:, 256 + c * 128:256 + (c + 1) * 128], lsp.t[:, c * 128:(c + 1) * 128], tri_incl),
                     reads=[lsp.b, cf.b], writes=[b_czb])
            p.op("pe", MM(CR.t[:, 0:256], tri_rev, lsp.t[:, :]), reads=[lsp.b, cf.b], writes=[CR.b])
            p.op("act", ACT(epos.t[:], CZ.t[:, 256:512], AF.Exp, bias=float(np.log(0.125))), reads=[b_czb],
                 writes=[epos.b])
            p.op("act", ACT(eneg.t[:], CZ.t[:, 256:512], AF.Exp, scale=-1.0), reads=[b_czb], writes=[eneg.b])
            p.op("act", ACT(dec.t[:], CZ.t[:, 256:512].rearrange("p (c t) -> p c t", c=2)[:, :, 127], AF.Exp),
                 reads=[b_czb], writes=[dec.b])
            p.op("act", ACT(erev.t[:], CR.t[:, 0:256], AF.Exp), reads=[CR.b], writes=[erev.b])
            for half in range(2):
                rows = slice(half * 64, (half + 1) * 64)
                p.op("dve", TT(qz.t[rows, half:4:2, :], qkT[par].t[rows, 0:2, :],
                               epos.t[rows, :].rearrange("p (c t) -> p c t", c=2), ALU.mult),
                     reads=[qkT[par].b, epos.b], writes=[qz.b])
            p.op("dve", TT(kdT.t[:], qkT[par].t[:, 2:4, :], eneg.t[:].rearrange("p (c t) -> p c t", c=2), ALU.mult),
                 reads=[qkT[par].b, eneg.b], writes=[kdT.b])
            p.op("pool", TT(kend.t[:], qk_tok[par].t[:, 256:512], erev.t[:], ALU.mult),
                 reads=[qk_tok[par].b, erev.b], writes=[kend.b])

        def y2(t):
            for h in range(4):
                p.op("pe", MM(CS3[:, h, :], kdT.t[:, h // 2, :], qz.t[:, h, :]), reads=[kdT.b, qz.b], writes=[CS.b])
            p.op("dve", TT(scT.t[:].rearrange("p h t -> p (h t)"), CS.t[:, :], cmask, ALU.mult),
                 reads=[CS.b, cf.b], writes=[scT.b])

        def y3(t):
            par = t % 2
            V = v_bf[par]
            S, S_bf = S2[t % 2], S_bf2[t % 2]
            if t // 2 == 0:
                p.op("pool", MSET(S.t[:], 0.0), writes=[S.b])
                p.op("pool", MSET(S_bf.t[:], 0.0), writes=[S_bf.b])
            for h in range(4):
                pr = h // 2
                p.op("pe", MM(CO.t[:, h * 128:(h + 1) * 128], scT.t[:, h, :], V.t[:, h * 128:(h + 1) * 128],
                              True, False), reads=[scT.b, V.b], writes=[CO.b])
                p.op("pe", MM(CO.t[:, h * 128:(h + 1) * 128], qz.t[:, h, :],
                              S_bf.t[:, pr * 128:(pr + 1) * 128], False, True),
                     reads=[qz.b, S_bf.b], writes=[CO.b])
            for pr in range(2):
                p.op("pe", MM(CR.t[:, pr * 256:(pr + 1) * 256], kend.t[:, pr * 128:(pr + 1) * 128],
                              V.t[:, pr * 256:(pr + 1) * 256]), reads=[kend.b, V.b], writes=[CR.b])
            for pr in range(2):
                for half in range(2):
                    rows = slice(half * 64, (half + 1) * 64)
                    p.op("dve", STT(S.t[rows, pr * 128:(pr + 1) * 128], S.t[rows, pr * 128:(pr + 1) * 128],
                                    dec.t[rows, pr:pr + 1],
                                    CR.t[rows, pr * 256 + half * 128:pr * 256 + (half + 1) * 128],
                                    ALU.mult, ALU.add), reads=[S.b, dec.b, CR.b], writes=[S.b])
            p.op("pool", CP(S_bf.t[:], S.t[:]), reads=[S.b], writes=[S_bf.b])
            p.op("act", ACT(sq.t[:], CO.t[:, :], AF.Square), reads=[CO.b], writes=[sq.b])
            p.op("dve", RED(hst.t[:, 0:4], sq.t[:].rearrange("p (h t) -> p h t", h=4), ALU.add), reads=[sq.b],
                 writes=[hst.b])
            p.op("act", ACT(hst.t[:, 4:8], hst.t[:, 0:4], AF.Ln, scale=1.0 / 128, bias=EPS), reads=[hst.b],
                 writes=[hst.b])
            p.op("act", ACT(hst.t[:, 8:12], hst.t[:, 4:8], AF.Exp, scale=-0.5), reads=[hst.b], writes=[hst.b])
            p.op("dve", TT(sq.t[:].rearrange("p (h t) -> p h t", h=4),
                           CO.t[:, :].rearrange("p (h t) -> p h t", h=4),
                           hst.t[:, 8:12].unsqueeze(2).to_broadcast([128, 4, 128]), ALU.mult),
                 reads=[CO.b, hst.b], writes=[sq.b])
            p.op("pool", TT(on_bf.t[:], sq.t[:], silu_r[par].t[:], ALU.mult), reads=[sq.b, silu_r[par].b],
                 writes=[on_bf.b])

        def y4(t):
            for c in range(4):
                p.op("pe", TR(pT.t[:, c * 128:(c + 1) * 128], on_bf.t[:, c * 128:(c + 1) * 128], ident),
                     reads=[on_bf.b, cb.b], writes=[pT.b])
            p.op("dve", CP(onT.t[:].rearrange("p c t -> p (c t)"), pT.t[:, 0:512]), reads=[pT.b], writes=[onT.b])

        def y5(t):
            G = sgg[t % 2]
            for hh in range(2):
                R = ring_next()
                for c in range(4):
                    p.op("pe", MM(R.t[:, :], onT.t[:, c, :], wgb.t[:, c, hh * 512:(hh + 1) * 512], c == 0, c == 3),
                         reads=[onT.b, wgb.b], writes=[R.b])
                p.op("dve", TT(G.t[:, hh * 512:(hh + 1) * 512], G.t[:, hh * 512:(hh + 1) * 512], R.t[:, :], ALU.mult),
                     reads=[G.b, R.b], writes=[G.b])

        def y6(t):
            j = t // 2
            ucur, uprev = u_bf[t % 4], u_bf[(t - 2) % 4]
            Am = poolA1 if j == 0 else poolA
            for g in range(4):
                p.op("pe", MM(CS3[:, g, :], ucur.t[:, g * 128:(g + 1) * 128], Am[:, g, :], True, j == 0),
                     reads=[ucur.b, cb.b], writes=[CS.b])
                if j > 0:
                    p.op("pe", MM(CS3[:, g, :], uprev.t[:, g * 128:(g + 1) * 128], poolB[:, g, :], False, True),
                         reads=[uprev.b, cb.b], writes=[CS.b])
            p.op("act", ACP(pm.t[:].rearrange("p g t -> p (g t)"), CS.t[:, :]), reads=[CS.b], writes=[pm.b])

        def y7(t):
            for g in range(4):
                p.op("pe", MM(CO.t[:, g * 128:(g + 1) * 128], poolw.t[:, g, :], pm.t[:, g, :]),
                     reads=[poolw.b, pm.b], writes=[CO.b])
            p.op("dve", CP(mix.t[:].rearrange("p g t -> p (g t)"), CO.t[:, :]), reads=[CO.b], writes=[mix.b])

        def y8(t):
            G, Pg = sgg[t % 2], sgp[t % 2]
            for hh in range(2):
                R = ring_next()
                for g in range(4):
                    p.op("pe", MM(R.t[:, :], mix.t[:, g, :], wpb.t[:, g, hh * 512:(hh + 1) * 512], g == 0, g == 3),
                         reads=[mix.b, wpb.b], writes=[R.b])
                p.op("dve", TT(Pg.t[:, hh * 512:(hh + 1) * 512], Pg.t[:, hh * 512:(hh + 1) * 512], R.t[:, :],
                               ALU.mult), reads=[Pg.b, R.b], writes=[Pg.b])
            p.op("pool", TT(mrg.t[:], G.t[:], Pg.t[:], ALU.add), reads=[G.b, Pg.b], writes=[mrg.b])

        def y9(t):
            for kc in range(8):
                p.op("pe", TR(pT.t[:, kc * 128:(kc + 1) * 128], mrg.t[:, kc * 128:(kc + 1) * 128], ident),
                     reads=[mrg.b, cb.b], writes=[pT.b])
            p.op("dve", CP(mT.t[:].rearrange("p k t -> p (k t)"), pT.t[:]), reads=[pT.b], writes=[mT.b])

        def y10(t):
            X = xs[t % 4]
            for hh in range(2):
                R = ring_next()
                for kc in range(8):
                    p.op("pe", MM(R.t[:, :], mT.t[:, kc, :], wout.t[:, kc, hh * 512:(hh + 1) * 512],
                                  kc == 0, kc == 7), reads=[mT.b, wout.b], writes=[R.b])
                p.op("dve", TT(x2s.t[:, hh * 512:(hh + 1) * 512], R.t[:, :], X.t[:, hh * 512:(hh + 1) * 512],
                               ALU.add), reads=[R.b, X.b], writes=[x2s.b])
            if stop == "p1":
                p.dma("sp", DMA(out_t[gt(t)], x2s.t[:]), reads=[x2s.b], writes=[b_out[gt(t)]], sem="x2o")
            else:
                p.dma("sp", DMA(x2_t[gt(t)], x2s.t[:]), reads=[x2s.b], writes=[b_x2[gt(t)]], sem="x2o")
            if t + 4 < ntr:
                xload(t + 4)

        def y_norm2(t):
            st = stt_[t % 2]
            p.op("act", ACT(h2b.t[:], x2s.t[:], AF.Square, accum_out=st.t[:, 3:4]), reads=[x2s.b],
                 writes=[h2b.b, st.b])
            p.op("act", ACT(st.t[:, 4:5], st.t[:, 3:4], AF.Ln, scale=1.0 / D, bias=EPS), reads=[st.b], writes=[st.b])
            p.op("act", ACT(st.t[:, 5:6], st.t[:, 4:5], AF.Exp, scale=-0.5), reads=[st.b], writes=[st.b])
            p.op("dve", STT(h2b.t[:], x2s.t[:], st.t[:, 5:6], g2b.t[:], ALU.mult, ALU.mult),
                 reads=[x2s.b, st.b, g2b.b], writes=[h2b.b])
            p.dma("sp", DMA(h2_t[gt(t)], h2b.t[:]), reads=[h2b.b], writes=[b_h2[gt(t)]], sem="h2o")

        def y11(t):
            for kc in range(8):
                p.op("pe", TR(pT.t[:, kc * 128:(kc + 1) * 128], h2b.t[:, kc * 128:(kc + 1) * 128], ident),
                     reads=[h2b.b, cb.b], writes=[pT.b])
            p.op("dve", CP(h2T.t[:].rearrange("p k t -> p (k t)"), pT.t[:]), reads=[pT.b], writes=[h2T.b])

        def y12(t):
            i = gt(t)
            RL = ring_next()
            for kc in range(8):
                p.op("pe", MM(RL.t[:, 0:36], h2T.t[:, kc, :], wr.t[:, kc, :], kc == 0, False),
                     reads=[h2T.b, wr.b], writes=[RL.b])
            p.op("pe", MM(RL.t[:, 0:36], ones_bf[0:1, :], brb.t[0:1, :], False, True), reads=[cb.b, brb.b],
                 writes=[RL.b])
            p.op("dve", CP(LG.t[:, i, :], RL.t[:, 0:36]), reads=[RL.b], writes=[LG.b])

        XA = (x_pre, x_hT, x_g1, x_g2, x_g1b, x_g3, x_g5)
        XB = (x_gr, x_gg0, x_gg1, x_gp0, x_gp1)
        Y1F = (y0, y1, y2, y3, y6, y7)
        for t_ in range(min(4, ntr)):
            xload(t_)
        for f_ in XA + XB:
            f_(0)
        if ntr > 1:
            for k_, f_ in enumerate(XA):
                f_(1)
                if k_ < len(Y1F):
                    Y1F[k_](0)
        else:
            for f_ in Y1F:
                f_(0)
        nconv = 0
        for m in range(ntr):
            hasXa = m + 2 < ntr
            hasXb = m + 1 < ntr
            hasY1 = m + 1 < ntr

            def XA_(f_):
                if hasXa:
                    f_(m + 2)

            def XB_(f_):
                if hasXb:
                    f_(m + 1)

            def Y1(f_):
                if hasY1:
                    f_(m + 1)
            XA_(x_pre)
            y4(m)
            Y1(y0)
            XB_(x_gr)
            y5(m)
            Y1(y1)
            y8(m)
            XB_(x_gg0)
            Y1(y6)
            XB_(x_gg1)
            Y1(y2)
            y9(m)
            Y1(y7)
            XB_(x_gp0)
            Y1(y3)
            XB_(x_gp1)
            XA_(x_hT)
            y10(m)
            y_norm2(m)
            XA_(x_g1)
            y11(m)
            XA_(x_g2)
            y12(m)
            XA_(x_g3)
            XA_(x_g1b)
            XA_(x_g5)
            if stop != "p1" and m < NXZ:
                p.dma("pool", DMA(xs_flat[:, m * 8192:(m + 1) * 8192], zeros_d), writes=[b_xz[m]], sem="xz%d" % m)
            if stop != "p1":
                for _ in range(2):
                    if nconv < NE * 3:
                        convert_dma(nconv // 3, nconv % 3)
                        nconv += 1
        if stop == "p1":
            p.barrier()
            s1.close()
            p.emit(top)
            return nc
        assert nconv == NE * 3

        p.barrier()
        s1.close()

        s15 = contextlib.ExitStack()
        Msum = T(s15, "Msum", [128, NT * 32], BF16)
        cntS = T(s15, "cntS", [128, NT, 32], F32)
        preS = T(s15, "preS", [128, NT, 32], F32)
        baseS = T(s15, "baseS", [128, NT, 32], F32)
        sm = T(s15, "sm", [128, 8, 32], F32)
        cmp1 = T(s15, "cmp1", [128, 32, 32], F32)
        cmp2 = T(s15, "cmp2", [128, NWB, 32], F32)
        destf = T(s15, "destf", [128, 2, NT], F32)
        bef = T(s15, "bef", [128, NWB], F32)
        h2r = [T(s15, "h2r%d" % i, [128, D], BF16) for i in range(4)]
        cps = T(s15, "cps", [128, NT * 32], F32, psum=True)
        pps = T(s15, "pps", [128, NT * 32], F32, psum=True)

        Mall = T(s15, "Mall", [128, 2, NT, 32], BF16)
        rg = T(s15, "rg", [128, 8, NT, 4], F32)
        re_ = T(s15, "re", [128, 4, NT, 8], F32)
        rs = T(s15, "rs", [128, 8, NT], F32)
        rbig = T(s15, "rbig", [128, NT, 32], F32)
        gl = LG.t[:, :, 0:4]
        el = LG.t[:, :, 4:36].rearrange("p n (g j) -> p n g j", g=4)
        gsub, goh = rg.t[:, 0], rg.t[:, 1]
        esel, oh1, esel2, oh2 = (re_.t[:, k_] for k_ in range(4))
        gmax, gsum, m1, m2, dd, e21, den = (rs.t[:, k_] for k_ in range(7))
        RB = [rg.b, re_.b, rs.b]
        p.op("dve", RED(gmax, gl, ALU.max), reads=[LG.b], writes=RB)
        p.op("dve", TT(gsub, gl, gmax.unsqueeze(2).to_broadcast([128, NT, 4]), ALU.subtract), reads=[LG.b] + RB,
             writes=RB)
        p.op("act", ACT(gsub, gsub, AF.Exp), reads=RB, writes=RB)
        p.op("dve", RED(gsum, gsub, ALU.add), reads=RB, writes=RB)
        p.op("dve", TT(goh, gl, gmax.unsqueeze(2).to_broadcast([128, NT, 4]), ALU.is_equal), reads=[LG.b] + RB,
             writes=RB)
        p.op("dve", TT(rbig.t[:].rearrange("p n (g j) -> p n g j", g=4), el,
                       goh.unsqueeze(3).to_broadcast([128, NT, 4, 8]), ALU.mult), reads=[LG.b] + RB, writes=[rbig.b])
        p.op("dve", RED(esel, rbig.t[:].rearrange("p n (g j) -> p n j g", g=4), ALU.add), reads=[rbig.b], writes=RB)
        p.op("dve", RED(m1, esel, ALU.max), reads=RB, writes=RB)
        p.op("dve", TT(oh1, esel, m1.unsqueeze(2).to_broadcast([128, NT, 8]), ALU.is_equal), reads=RB, writes=RB)
        p.op("dve", STT(esel2, oh1, -1e30, esel, ALU.mult, ALU.add), reads=RB, writes=RB)
        p.op("dve", RED(m2, esel2, ALU.max), reads=RB, writes=RB)
        p.op("dve", TT(oh2, esel2, m2.unsqueeze(2).to_broadcast([128, NT, 8]), ALU.is_equal), reads=RB, writes=RB)
        p.op("dve", TT(dd, m2, m1, ALU.subtract), reads=RB, writes=RB)
        p.op("act", ACT(e21, dd, AF.Exp), reads=RB, writes=RB)
        p.op("dve", STT(den, e21, 1.0, gsum, ALU.add, ALU.mult), reads=RB, writes=RB)
        p.op("dve", RCP(Wall.t[:, :, 0], den), reads=RB, writes=[Wall.b])
        p.op("dve", TT(Wall.t[:, :, 1], Wall.t[:, :, 0], e21, ALU.mult), reads=RB + [Wall.b], writes=[Wall.b])
        for k_, ohk in ((0, oh1), (1, oh2)):
            p.op("dve", TT(Mall.t[:, k_].rearrange("p n (g j) -> p n g j", g=4),
                           goh.unsqueeze(3).to_broadcast([128, NT, 4, 8]),
                           ohk.unsqueeze(2).to_broadcast([128, NT, 4, 8]), ALU.mult), reads=RB, writes=[Mall.b])
        p.op("pool", TT(Msum.t[:], Mall.t[:, 0].rearrange("p n e -> p (n e)"),
                        Mall.t[:, 1].rearrange("p n e -> p (n e)"), ALU.add), reads=[Mall.b], writes=[Msum.b])
        for q in range(4):
            p.op("pe", MM(cps.t[:, q * 512:(q + 1) * 512], ones_bf, Msum.t[:, q * 512:(q + 1) * 512]),
                 reads=[cb.b, Msum.b], writes=[cps.b])
            p.op("pe", MM(pps.t[:, q * 512:(q + 1) * 512], lstrict, Msum.t[:, q * 512:(q + 1) * 512]),
                 reads=[cb.b, Msum.b], writes=[pps.b])
        p.op("act", ACP(cntS.t[:].rearrange("p n e -> p (n e)"), cps.t[:]), reads=[cps.b], writes=[cntS.b])
        p.op("dve", CP(preS.t[:].rearrange("p n e -> p (n e)"), pps.t[:]), reads=[pps.b], writes=[preS.b])
        p.op("dve", MSET(baseS.t[:, 0, :], 0.0), writes=[baseS.b])
        for i in range(1, NT):
            p.op("dve", TT(baseS.t[:, i, :], baseS.t[:, i - 1, :], cntS.t[:, i - 1, :], ALU.add),
                 reads=[baseS.b, cntS.b], writes=[baseS.b])
        tot, nblk, padded, pend, pstart, ones32 = (sm.t[:, k, :] for k in range(6))
        smb = [sm.b]
        p.op("dve", TT(tot, baseS.t[:, NT - 1, :], cntS.t[:, NT - 1, :], ALU.add), reads=[baseS.b, cntS.b], writes=smb)
        p.op("dve", TT(cmp1.t[:], tot.unsqueeze(2).to_broadcast([128, 32, 32]),
                       thr[:, 0:64:2].unsqueeze(1).to_broadcast([128, 32, 32]), ALU.is_gt), reads=smb + [cf.b],
             writes=[cmp1.b])
        p.op("dve", RED(nblk, cmp1.t[:], ALU.add), reads=[cmp1.b], writes=smb)
        p.op("dve", TS(padded, nblk, 256.0, ALU.mult), reads=smb, writes=smb)
        p.op("dve", MSET(ones32, 1.0), writes=smb)
        p.op("dve", lambda e: e.tensor_tensor_scan(out=pend, data0=ones32, data1=padded, initial=0.0,
                                                   op0=ALU.mult, op1=ALU.add), reads=smb, writes=smb)
        p.op("dve", TT(pstart, pend, padded, ALU.subtract), reads=smb, writes=smb)
        p.op("dve", TT(preS.t[:], preS.t[:], baseS.t[:], ALU.add), reads=[preS.b, baseS.b], writes=[preS.b])
        p.op("dve", TT(preS.t[:], preS.t[:], pstart.unsqueeze(1).to_broadcast([128, NT, 32]), ALU.add),
             reads=[preS.b] + smb, writes=[preS.b])
        for k in range(2):
            p.op("dve", TT(baseS.t[:], Mall.t[:, k], preS.t[:], ALU.mult), reads=[Mall.b, preS.b], writes=[baseS.b])
            p.op("dve", RED(destf.t[:, k, :], baseS.t[:], ALU.add), reads=[baseS.b], writes=[destf.b])
        p.op("dve", CP(desti.t[:], destf.t[:]), reads=[destf.b], writes=[desti.b])
        p.op("dve", TT(cmp2.t[:], pend.unsqueeze(1).to_broadcast([128, NWB, 32]),
                       bvals.unsqueeze(2).to_broadcast([128, NWB, 32]), ALU.is_le), reads=smb + [cf.b],
             writes=[cmp2.b])
        p.op("dve", RED(bef.t[:], cmp2.t[:], ALU.add), reads=[cmp2.b], writes=[bef.b])
        p.op("dve", TS(bef.t[:], bef.t[:], 31.0, ALU.min), reads=[bef.b], writes=[bef.b])
        p.op("dve", TS(bef.t[:], bef.t[:], 128.0, ALU.mult, pidx, ALU.add), reads=[bef.b, cf.b], writes=[bef.b])
        p.op("dve", CP(widxi.t[:], bef.t[:]), reads=[bef.b], writes=[widxi.b])
        def h2load(i):
            Hr = h2r[i % 4]
            p.dma("sp", DMA(Hr.t[:], h2_t[i]), reads=[b_h2[i]], writes=[Hr.b], sem="h2l%d" % (i % 4))
        for i in range(3):
            h2load(i)
        for i in range(NT):
            Hr = h2r[i % 4]
            if i + 3 < NT:
                h2load(i + 3)
            for k in range(2):
                p.dma("pool", SCAT(xs_d, desti.t[:, k, i:i + 1], Hr.t[:]), reads=[Hr.b, desti.b] + b_xz,
                      writes=[b_sc[2 * i + k]], sem="sc%d%d" % (i % 4, k))
        p.barrier()
        s15.close()

        s2 = contextlib.ExitStack()
        NBX, NBW = 5, 3
        NBX2 = 3
        xb = [T(s2, "xb%d" % i, [128, 2, D], BF16) for i in range(NBX2)]
        xs_blk = xs_d.rearrange("(n h p) d -> n p h d", h=2, p=128)
        y_blk = y_d.rearrange("(n h p) d -> n p h d", h=2, p=128)
        wall = [T(s2, "walls%d" % i, [128, 6144], BF16) for i in range(NBW)]
        xbT = [T(s2, "xbT%d" % i, [128, 8, 128], BF16) for i in range(2)]
        sgm = [T(s2, "sgm%d" % i, [128, 256], F32) for i in range(2)]
        a_bf = [T(s2, "a_bf%d" % i, [128, 256], BF16) for i in range(2)]
        aT = [T(s2, "aT%d" % i, [128, 2, 128], BF16) for i in range(2)]
        ysb = [T(s2, "ysb%d" % i, [128, 2, D], F32) for i in range(2)]
        pTa = T(s2, "pTa", [128, D], BF16, psum=True)
        pTc = T(s2, "pTc", [128, 256], BF16, psum=True)
        pH = [T(s2, "pH%d" % i, [128, 512], F32, psum=True) for i in range(2)]
        pY = [T(s2, "pY%d" % i, [128, D], F32, psum=True) for i in range(2)]

        def xb_load(wb):
            r = wb % NBX2
            p.dma("sp", DMA(xb[r].t[:], xs_blk[wb]), reads=b_sc, writes=[xb[r].b], sem="xbl%d" % r)

        def w_load(wb):
            r = wb % NBW
            p.dma("pool", GATH(wall[r].t[:], wall_d, widxi.t[:, wb:wb + 1]), reads=b_wgu + b_wd + [widxi.b],
                  writes=[wall[r].b], sem="wgl%d" % r)

        def stA(b):
            q, X_ = b % 2, xb[(b // 2) % NBX2]
            for kc in range(8):
                p.op("pe", TR(pTa.t[:, kc * 128:(kc + 1) * 128], X_.t[:, b % 2, kc * 128:(kc + 1) * 128], ident),
                     reads=[X_.b, cb.b], writes=[pTa.b])
            p.op("dve", CP(xbT[q].t[:].rearrange("p k t -> p (k t)"), pTa.t[:]), reads=[pTa.b], writes=[xbT[q].b])

        def stB(b):
            q, W_ = b % 2, wall[(b // 2) % NBW]
            w3 = W_.t[:, 0:4096].rearrange("p (k n) -> p k n", k=8)
            for kc in range(8):
                p.op("pe", MM(pH[q].t[:, :], xbT[q].t[:, kc, :], w3[:, kc, :], kc == 0, kc == 7),
                     reads=[xbT[q].b, W_.b], writes=[pH[q].b])
            p.op("act", ACT(sgm[q].t[:], pH[q].t[:, 0:256], AF.Sigmoid), reads=[pH[q].b], writes=[sgm[q].b])
            p.op("dve", TT(sgm[q].t[:], sgm[q].t[:], pH[q].t[:, 0:256], ALU.mult), reads=[sgm[q].b, pH[q].b],
                 writes=[sgm[q].b])
            p.op("dve", TT(a_bf[q].t[:], sgm[q].t[:], pH[q].t[:, 256:512], ALU.mult), reads=[sgm[q].b, pH[q].b],
                 writes=[a_bf[q].b])

        def stC(b):
            q = b % 2
            for c in range(2):
                p.op("pe", TR(pTc.t[:, c * 128:(c + 1) * 128], a_bf[q].t[:, c * 128:(c + 1) * 128], ident),
                     reads=[a_bf[q].b, cb.b], writes=[pTc.b])
            p.op("act", ACP(aT[q].t[:].rearrange("p c t -> p (c t)"), pTc.t[:, :]), reads=[pTc.b], writes=[aT[q].b])

        def stD(b):
            q, W_ = b % 2, wall[(b // 2) % NBW]
            d3 = W_.t[:, 4096:6144].rearrange("p (c n) -> p c n", c=2)
            for hh in range(2):
                for c in range(2):
                    p.op("pe", MM(pY[q].t[:, hh * 512:(hh + 1) * 512], aT[q].t[:, c, :],
                                  d3[:, c, hh * 512:(hh + 1) * 512], c == 0, c == 1),
                         reads=[aT[q].b, W_.b], writes=[pY[q].b])
            Yb = ysb[(b // 2) % 2]
            p.op("act", ACP(Yb.t[:, b % 2, 0:512], pY[q].t[:, 0:512]), reads=[pY[q].b], writes=[Yb.b])
            p.op("dve", CP(Yb.t[:, b % 2, 512:1024], pY[q].t[:, 512:1024]), reads=[pY[q].b], writes=[Yb.b])
            if b % 2 == 1:
                p.dma("sp", DMA(y_blk[b // 2], Yb.t[:]), reads=[Yb.b], writes=[b_y[b - 1], b_y[b]],
                      sem="yo%d" % ((b // 2) % 2))

        for b in range(3):
            xb_load(b)
        for b in range(2):
            w_load(b)
        stA(0)
        stA(1)
        stB(0)
        for i in range(NBLK):
            if i % 2 == 0 and i // 2 + 3 < NWB:
                xb_load(i // 2 + 3)
            if i % 2 == 0 and i // 2 + 2 < NWB:
                w_load(i // 2 + 2)
            stC(i)
            if i + 2 < NBLK:
                stA(i + 2)
            if i + 1 < NBLK:
                stB(i + 1)
            stD(i)
        p.barrier()
        s2.close()

        s3 = contextlib.ExitStack()
        NB3 = 3
        NP3 = NT // 2
        xa = [T(s3, "xa%d" % i, [128, 2, D], F32) for i in range(NB3)]
        y0 = [T(s3, "y0%d" % i, [128, D], F32) for i in range(4)]
        y1 = [T(s3, "y1%d" % i, [128, D], F32) for i in range(4)]
        ob = [T(s3, "ob%d" % i, [128, 2, D], F32) for i in range(2)]
        junk3 = T(s3, "junk3", [128, D], BF16)
        st3_ = [T(s3, "st3%d" % i, [128, 4], F32) for i in range(2)]
        x2_pair = x2_d.rearrange("(n h p) d -> n p h d", h=2, p=128)
        out_pair = out_d.rearrange("(n h p) d -> n p h d", h=2, p=128)

        def p3_xload(j):
            r = j % NB3
            p.dma("sp", DMA(xa[r].t[:], x2_pair[j]), reads=[b_x2[2 * j], b_x2[2 * j + 1]], writes=[xa[r].b],
                  sem="x2l%d" % r)

        def p3_gath(i):
            r = i % 4
            p.dma("pool", GATH(y0[r].t[:], y_d, desti.t[:, 0, i:i + 1]), reads=b_y + [desti.b], writes=[y0[r].b],
                  sem="y0l%d" % r)
            p.dma("pool", GATH(y1[r].t[:], y_d, desti.t[:, 1, i:i + 1]), reads=b_y + [desti.b], writes=[y1[r].b],
                  sem="y1l%d" % r)
        p3_xload(0)
        p3_xload(1)
        for i_ in range(3):
            p3_gath(i_)
        for i in range(NT):
            j, h = i // 2, i % 2
            if h == 0 and j + 2 < NP3:
                p3_xload(j + 2)
            if i + 3 < NT:
                p3_gath(i + 3)
            A = xa[j % NB3].t[:, h, :]
            Ab = xa[j % NB3].b
            Y0, Y1, stq = y0[i % 4], y1[i % 4], st3_[i % 2]
            Ob = ob[j % 2]
            O = Ob.t[:, h, :]
            p.op("dve", STT(A, Y0.t[:], Wall.t[:, i, 0:1], A, ALU.mult, ALU.add),
                 reads=[Y0.b, Wall.b, Ab], writes=[Ab])
            p.op("dve", STT(A, Y1.t[:], Wall.t[:, i, 1:2], A, ALU.mult, ALU.add),
                 reads=[Y1.b, Wall.b, Ab], writes=[Ab])
            p.op("act", ACT(junk3.t[:], A, AF.Square, accum_out=stq.t[:, 0:1]), reads=[Ab],
                 writes=[junk3.b, stq.b])
            p.op("act", ACT(stq.t[:, 1:2], stq.t[:, 0:1], AF.Ln, scale=1.0 / D, bias=EPS), reads=[stq.b],
                 writes=[stq.b])
            p.op("act", ACT(stq.t[:, 2:3], stq.t[:, 1:2], AF.Exp, scale=-0.5), reads=[stq.b], writes=[stq.b])
            p.op("dve", STT(O, A, stq.t[:, 2:3], gf.t[:], ALU.mult, ALU.mult),
                 reads=[Ab, stq.b, gf.b], writes=[Ob.b])
            if h == 1:
                p.dma("sp", DMA(out_pair[j], Ob.t[:]), reads=[Ob.b], writes=[b_out[i - 1], b_out[i]],
                      sem="oo%d" % (j % 2))
        p.wait_only("sp", reads=b_out)
        s3.close()
        p.emit(top)
    return nc


def _constants():
    s = np.arange(128)[:, None]
    t = np.arange(128)[None, :]
    cf = np.zeros((128, NCF), np.float32)
    cf[:, CF_TRI:CF_TRI + 128] = np.where(s <= t, -1.0 / 16.0, 0.0)
    cf[:, CF_REV:CF_REV + 128] = np.where(s > t, -1.0 / 16.0, 0.0)
    cf[:, CF_MASK:CF_MASK + 512] = np.tile(np.where(s <= t, 1.0, 0.0), (1, 4))
    cf[:, CF_THR:CF_THR + 64] = 128.0 * np.arange(64)[None, :]
    cf[:, CF_BV:CF_BV + NWB] = 256.0 * np.arange(NWB)[None, :]
    cf[:, CF_PIDX] = np.arange(128)
    cb = np.zeros((128, NCB), np.float32)
    cb[:, CB_ID:CB_ID + 128] = np.eye(128)
    cb[:, CB_ONES:CB_ONES + 128] = 1.0
    cb[:, CB_LS:CB_LS + 128] = np.where(s < t, 1.0, 0.0)
    for g, w in enumerate((2, 4, 8, 16)):
        A = np.where((s <= t) & (s > t - w), 1.0 / w, 0.0) - np.where(s == t, 1.0, 0.0)
        B = np.where(s - 128 > t - w, 1.0 / w, 0.0)
        cnt = np.minimum(t + 1, w).astype(np.float64)
        A1 = np.where((s <= t) & (s > t - w), 1.0 / cnt, 0.0) - np.where(s == t, 1.0, 0.0)
        cb[:, CB_A + g * 128:CB_A + (g + 1) * 128] = A
        cb[:, CB_B + g * 128:CB_B + (g + 1) * 128] = B
        cb[:, CB_A1 + g * 128:CB_A1 + (g + 1) * 128] = A1
    return cf, cb.astype(ml_dtypes.bfloat16)


_NC = None


def kernel(x, norm1_g, w_in, w_alpha_up, b_alpha, gla_norm_g, w_gla_branch, pool_w, pool_scale,
           w_pool_branch, w_out, norm2_g, w_router_group, b_router_group, w_router_expert,
           b_router_expert, w_exp_gate, w_exp_up, w_exp_down, norm_f_g):
    global _NC
    f = lambda a: np.ascontiguousarray(np.asarray(a), dtype=np.float32)
    x = f(x)
    cf, cb = _constants()
    cols = np.concatenate([
        f(norm1_g)[0].reshape(8, 128).T, f(norm2_g)[0].reshape(8, 128).T,
        f(gla_norm_g)[0].reshape(4, 128).T, f(pool_scale)[0].reshape(4, 128).T], axis=1)
    wr = np.concatenate([f(w_router_group)[0], f(w_router_expert)[0].transpose(1, 0, 2).reshape(D, 32)], axis=1)
    br = np.concatenate([f(b_router_group)[0], f(b_router_expert)[0].reshape(32)])[None, :]
    wa = np.concatenate([f(w_alpha_up)[0], f(b_alpha)[0][None, :]], axis=0)
    shared = dict(
        w_in=f(w_in)[0], wa=f(wa), cols=f(cols), wgb=f(w_gla_branch)[0], wpb=f(w_pool_branch)[0],
        wout=f(w_out)[0], poolw=f(pool_w)[0], wr=f(wr), br=f(br), weg=f(w_exp_gate)[0], weu=f(w_exp_up)[0],
        wed=f(w_exp_down)[0], gf=f(norm_f_g), g2=f(norm2_g)[0], cf=cf, cb=cb,
        zeros=np.zeros((128, 8192), ml_dtypes.bfloat16))
    if _NC is None:
        _NC = build_program()
    in_maps = []
    for c in range(NCORES):
        m = dict(shared)
        m["x"] = np.ascontiguousarray(x[2 * c:2 * c + 2].reshape(TOK, D))
        in_maps.append(m)
    res = run_bass_kernel_spmd(_NC, in_maps, core_ids=list(range(NCORES)))
    out = np.concatenate([np.asarray(r["out"]).reshape(2, SEQ, D) for r in res.results], axis=0)
    return out.astype(np.float32)
```

```python
import contextlib
import numpy as np
import ml_dtypes
import concourse.bass as bass
import concourse.mybir as mybir
from concourse.bass_utils import run_bass_kernel_spmd

F32 = mybir.dt.float32
BF16 = mybir.dt.bfloat16
I32 = mybir.dt.int32
AF = mybir.ActivationFunctionType
ALU = mybir.AluOpType
AX = mybir.AxisListType

NCORES = 8
D = 1024
SEQ = 4096
TOK = 8192
NT = 64
TPS = 32
DIN = 4112
NE = 32
NWB = 96
NBLK = 2 * NWB
NROWS = NBLK * 128
EPS = 1e-6

ENGINES = ("sp", "act", "dve", "pool", "pe")
STRICT = {"act": True, "dve": True, "pool": True, "pe": False, "sp": False}


class Buf:
    __slots__ = ("name", "lw", "rd")

    def __init__(self, name):
        self.name = name
        self.lw = None
        self.rd = []


class Prog:
    def __init__(self, nc):
        self.nc = nc
        self.ops = {e: [] for e in ENGINES}
        self.clk = {e: {} for e in ENGINES}
        self.waited_own = {e: 0 for e in ENGINES}
        self.opclock = {}
        self.dma_cnt = {}
        self.dma_last = {}
        self.signal = set()
        self.max_ops = None
        self.nrec = 0

    def _over(self):
        self.nrec += 1
        return self.max_ops is not None and self.nrec > self.max_ops

    def _resolve(self, eng, is_dma, reads, writes, extra):
        deps = set(extra)
        for b in reads:
            if b.lw is not None:
                deps.add(b.lw)
        for b in writes:
            if b.lw is not None:
                deps.add(b.lw)
            deps.update(b.rd)
        clk = self.clk[eng]
        waits = []
        for tok in sorted(deps, key=lambda t: (t[0], t[1], -t[2])):
            k = (tok[0], tok[1])
            if tok[0] == "c" and tok[1] == eng:
                if not (is_dma or STRICT[eng]):
                    continue
                if self.waited_own[eng] >= tok[2]:
                    continue
                self.waited_own[eng] = tok[2]
                waits.append(tok)
                self.signal.add(tok)
                continue
            if clk.get(k, 0) >= tok[2]:
                continue
            waits.append(tok)
            if tok[0] == "c":
                self.signal.add(tok)
            oc = self.opclock.get(tok)
            if oc:
                for kk, vv in oc.items():
                    if kk == ("c", eng):
                        continue
                    if clk.get(kk, 0) < vv:
                        clk[kk] = vv
            if clk.get(k, 0) < tok[2]:
                clk[k] = tok[2]
        return waits

    def _finish(self, tok, reads, writes):
        for b in reads:
            b.rd.append(tok)
        for b in writes:
            b.lw = tok
            b.rd = []

    def op(self, eng, fn, reads=(), writes=(), extra=()):
        if self._over():
            return None
        waits = self._resolve(eng, False, reads, writes, extra)
        lst = self.ops[eng]
        idx = len(lst) + 1
        tok = ("c", eng, idx)
        lst.append(dict(fn=fn, waits=waits, dma=None))
        self.clk[eng][("c", eng)] = idx
        self.opclock[tok] = dict(self.clk[eng])
        self._finish(tok, reads, writes)
        return tok

    def dma(self, eng, fn, reads=(), writes=(), sem=None, extra=()):
        if self._over():
            return None
        ex = list(extra)
        if sem in self.dma_last:
            ex.append(self.dma_last[sem])
        waits = self._resolve(eng, True, reads, writes, ex)
        cnt = self.dma_cnt.get(sem, 0) + 16
        self.dma_cnt[sem] = cnt
        tok = ("d", sem, cnt)
        self.dma_last[sem] = tok
        lst = self.ops[eng]
        idx = len(lst) + 1
        lst.append(dict(fn=fn, waits=waits, dma=sem))
        self.clk[eng][("c", eng)] = idx
        self.opclock[tok] = dict(self.clk[eng])
        self._finish(tok, reads, writes)
        return tok

    def wait_only(self, eng, reads=(), writes=(), extra=()):
        waits = self._resolve(eng, True, reads, writes, extra)
        lst = self.ops[eng]
        lst.append(dict(fn=None, waits=waits, dma=None))
        self.clk[eng][("c", eng)] = len(lst)

    def barrier(self):
        toks = []
        for e in ENGINES:
            for i in range(len(self.ops[e]), 0, -1):
                o = self.ops[e][i - 1]
                if o["fn"] is not None and o["dma"] is None:
                    toks.append(("c", e, i))
                    break
        toks.extend(self.dma_last.values())
        for e in ENGINES:
            self.wait_only(e, extra=toks)

    def emit(self, stack):
        nc = self.nc
        esem = {e: stack.enter_context(nc.semaphore("es_" + e)) for e in ENGINES}
        dsem = {k: stack.enter_context(nc.semaphore("ds_%d" % i)) for i, k in enumerate(self.dma_cnt)}
        signum = {}
        for e in ENGINES:
            c = 0
            for i, o in enumerate(self.ops[e]):
                tok = ("c", e, i + 1)
                if tok in self.signal:
                    assert o["fn"] is not None and o["dma"] is None, (e, i)
                    c += 1
                    signum[tok] = c

        def run(e, engobj):
            for i, o in enumerate(self.ops[e]):
                for w in o["waits"]:
                    if w[0] == "c":
                        engobj.wait_ge(esem[w[1]], signum[w])
                    else:
                        engobj.wait_ge(dsem[w[1]], w[2])
                if o["fn"] is None:
                    continue
                inst = o["fn"](engobj)
                if o["dma"] is not None:
                    inst.then_inc(dsem[o["dma"]], 16)
                elif ("c", e, i + 1) in signum:
                    inst.then_inc(esem[e], 1)

        with nc.Block() as block:
            @block.sync
            def _(x):
                run("sp", x)

            @block.scalar
            def _(x):
                run("act", x)

            @block.vector
            def _(x):
                run("dve", x)

            @block.gpsimd
            def _(x):
                run("pool", x)

            @block.tensor
            def _(x):
                run("pe", x)


def MM(out, lhsT, rhs, start=True, stop=True):
    return lambda e: e.matmul(out, lhsT=lhsT, rhs=rhs, start=start, stop=stop)


def TR(out, in_, ident):
    return lambda e: e.transpose(out=out, in_=in_, identity=ident)


def TT(out, in0, in1, op):
    return lambda e: e.tensor_tensor(out=out, in0=in0, in1=in1, op=op)


def TS(out, in0, s1, op0, s2=None, op1=None):
    if op1 is None:
        return lambda e: e.tensor_scalar(out=out, in0=in0, scalar1=s1, scalar2=None, op0=op0)
    return lambda e: e.tensor_scalar(out=out, in0=in0, scalar1=s1, scalar2=s2, op0=op0, op1=op1)


def STT(out, in0, scalar, in1, op0, op1):
    return lambda e: e.scalar_tensor_tensor(out=out, in0=in0, scalar=scalar, in1=in1, op0=op0, op1=op1)


def ACT(out, in_, func, **kw):
    return lambda e: e.activation(out=out, in_=in_, func=func, **kw)


def CP(out, in_):
    return lambda e: e.tensor_copy(out=out, in_=in_)


def ACP(out, in_):
    return lambda e: e.copy(out=out, in_=in_)


def RED(out, in_, op):
    return lambda e: e.tensor_reduce(out=out, in_=in_, axis=AX.X, op=op)


def RCP(out, in_):
    return lambda e: e.reciprocal(out=out, in_=in_)


def MSET(ap, v):
    return lambda e: e.memset(ap, v)


def DMA(out, in_):
    return lambda e: e.dma_start(out=out, in_=in_)


def SCAT(out, idx, in_):
    return lambda e: e.indirect_dma_start(out=out, out_offset=bass.IndirectOffsetOnAxis(ap=idx, axis=0),
                                          in_=in_, in_offset=None)


def GATH(out, in_, idx):
    return lambda e: e.indirect_dma_start(out=out, out_offset=None, in_=in_,
                                          in_offset=bass.IndirectOffsetOnAxis(ap=idx, axis=0))


CF_TRI, CF_REV, CF_MASK, CF_THR, CF_BV, CF_PIDX, NCF = 0, 128, 256, 768, 832, 928, 929
CB_ID, CB_ONES, CB_LS, CB_A, CB_B, CB_A1, NCB = 0, 128, 256, 384, 896, 1408, 1920


def build_program(ntr=NT, stop=None, max_ops=None):
    nc = bass.Bass("TRN2", target_bir_lowering=False)

    def din(name, shape, dt=F32):
        return nc.dram_tensor(name, shape, dt, kind="ExternalInput").ap()

    def dscr(name, shape, dt):
        return nc.dram_tensor(name, shape, dt, kind="Internal").ap()

    x_d = din("x", [TOK, D])
    w_in_d = din("w_in", [D, DIN])
    wa_d = din("wa", [17, 256])
    cols_d = din("cols", [128, 24])
    wgb_d = din("wgb", [512, D])
    wpb_d = din("wpb", [512, D])
    wout_d = din("wout", [D, D])
    poolw_d = din("poolw", [4, 128, 128])
    wr_d = din("wr", [D, 36])
    br_d = din("br", [1, 36])
    weg_d = din("weg", [NE, D, 256])
    weu_d = din("weu", [NE, D, 256])
    wed_d = din("wed", [NE, 256, D])
    gf_d = din("gf", [D])
    g2_d = din("g2", [D])
    cf_d = din("cf", [128, NCF])
    cb_d = din("cb", [128, NCB], BF16)
    zeros_d = din("zeros", [128, 8192], BF16)
    out_d = nc.dram_tensor("out", [TOK, D], F32, kind="ExternalOutput").ap()

    x2_d = dscr("x2s", [TOK, D], F32)
    h2_d = dscr("h2s", [TOK, D], BF16)
    xs_d = dscr("xsort", [NROWS, D], BF16)
    y_d = dscr("ysort", [NROWS, D], F32)
    wall_d = dscr("wall", [NE * 128, 8 * 512 + 2 * 1024], BF16)

    top = contextlib.ExitStack()
    with top:
        p = Prog(nc)
        p.max_ops = max_ops

        class T:
            def __init__(self, stack, name, shape, dt, psum=False):
                if psum:
                    self.t = stack.enter_context(nc.psum_tensor("t_" + name, shape, dt))
                else:
                    self.t = stack.enter_context(nc.sbuf_tensor("t_" + name, shape, dt))
                self.b = Buf(name)

        cf = T(top, "cf", [128, NCF], F32)
        cb = T(top, "cb", [128, NCB], BF16)
        cols = T(top, "cols", [128, 24], F32)
        gf = T(top, "gf", [128, D], F32)
        LG = T(top, "LG", [128, NT, 36], F32)
        Wall = T(top, "Wall", [128, NT, 2], F32)
        desti = T(top, "desti", [128, 2, NT], I32)
        widxi = T(top, "widxi", [128, NWB], I32)

        p.dma("sp", DMA(cf.t[:], cf_d), writes=[cf.b], sem="c0")
        p.dma("sp", DMA(cb.t[:], cb_d), writes=[cb.b], sem="c1")
        p.dma("sp", DMA(cols.t[:], cols_d), writes=[cols.b], sem="c2")
        p.dma("act", DMA(gf.t[:], gf_d.partition_broadcast(128)), writes=[gf.b], sem="c3")

        ident = cb.t[:, CB_ID:CB_ID + 128]
        ones_bf = cb.t[:, CB_ONES:CB_ONES + 128]
        lstrict = cb.t[:, CB_LS:CB_LS + 128]
        poolA = cb.t[:, CB_A:CB_A + 512].rearrange("p (g t) -> p g t", g=4)
        poolB = cb.t[:, CB_B:CB_B + 512].rearrange("p (g t) -> p g t", g=4)
        poolA1 = cb.t[:, CB_A1:CB_A1 + 512].rearrange("p (g t) -> p g t", g=4)
        tri_incl = cf.t[:, CF_TRI:CF_TRI + 128]
        tri_rev = cf.t[:, CF_REV:CF_REV + 128]
        cmask = cf.t[:, CF_MASK:CF_MASK + 512]
        thr = cf.t[:, CF_THR:CF_THR + 64]
        bvals = cf.t[:, CF_BV:CF_BV + NWB]
        pidx = cf.t[:, CF_PIDX:CF_PIDX + 1]
        g1col = cols.t[:, 0:8]
        g2col = cols.t[:, 8:16]
        glncol = cols.t[:, 16:20]
        psccol = cols.t[:, 20:24]

        x_t = x_d.rearrange("(n p) d -> n p d", p=128)
        out_t = out_d.rearrange("(n p) d -> n p d", p=128)
        x2_t = x2_d.rearrange("(n p) d -> n p d", p=128)
        h2_t = h2_d.rearrange("(n p) d -> n p d", p=128)
        xs_t = xs_d.rearrange("(n p) d -> n p d", p=128)
        y_t = y_d.rearrange("(n p) d -> n p d", p=128)
        wall_t = wall_d.rearrange("(e p) f -> e p f", p=128)
        b_x2 = [Buf("x2d%d" % i) for i in range(NT)]
        b_h2 = [Buf("h2d%d" % i) for i in range(NT)]
        b_sc = [Buf("scd%d" % i) for i in range(2 * NT)]
        b_y = [Buf("yd%d" % i) for i in range(NBLK)]
        b_wgu = [Buf("wgud%d" % i) for i in range(NE)]
        b_wd = [Buf("wdd%d" % i) for i in range(NE)]
        b_out = [Buf("outd%d" % i) for i in range(NT)]
        NXZ = NROWS * D // (128 * 8192)
        b_xz = [Buf("xz%d" % i) for i in range(NXZ)]
        xs_flat = xs_d.rearrange("(p r) d -> p (r d)", p=128)

        s1 = contextlib.ExitStack()
        w_in = T(s1, "w_in", [128, 8, DIN], BF16)
        wgb = T(s1, "wgbb", [128, 4, D], BF16)
        wpb = T(s1, "wpbb", [128, 4, D], BF16)
        wout = T(s1, "woutb", [128, 8, D], BF16)
        poolw = T(s1, "poolwb", [128, 4, 128], BF16)
        wr = T(s1, "wrb", [128, 8, 36], BF16)
        brb = T(s1, "brb", [1, 36], BF16)
        wa = T(s1, "wa", [17, 256], F32)
        g2b = T(s1, "g2b", [128, D], F32)

        xs = [T(s1, "xs%d" % i, [128, D], F32) for i in range(4)]
        stt_ = [T(s1, "st%d" % i, [128, 16], F32) for i in range(2)]
        h_bf = T(s1, "h_bf", [128, D], BF16)
        hT = T(s1, "hT", [128, 8, 128], BF16)
        qk_tok = [T(s1, "qk_tok%d" % i, [128, 512], BF16) for i in range(2)]
        qkT = [T(s1, "qkT%d" % i, [128, 4, 128], BF16) for i in range(2)]
        zl = [T(s1, "zl%d" % i, [32, 128], F32) for i in range(2)]
        v_bf = [T(s1, "v_bf%d" % i, [128, 512], BF16) for i in range(2)]
        u_bf = [T(s1, "u_bf%d" % i, [128, 512], BF16) for i in range(4)]
        silu_r = [T(s1, "silu_r%d" % i, [128, 512], F32) for i in range(2)]
        r_sb = T(s1, "r_sb", [128, 512], F32)
        sgg = [T(s1, "sgg%d" % i, [128, D], F32) for i in range(2)]
        sgp = [T(s1, "sgp%d" % i, [128, D], F32) for i in range(2)]
        lsp = T(s1, "lsp", [128, 256], F32)
        epos = T(s1, "epos", [128, 256], F32)
        eneg = T(s1, "eneg", [128, 256], F32)
        erev = T(s1, "erev", [128, 256], F32)
        dec = T(s1, "dec", [128, 2], F32)
        qz = T(s1, "qz", [128, 4, 128], BF16)
        kdT = T(s1, "kdT", [128, 2, 128], BF16)
        kend = T(s1, "kend", [128, 256], BF16)
        scT = T(s1, "scT", [128, 4, 128], BF16)
        S2 = [T(s1, "S%d" % i, [128, 256], F32) for i in range(2)]
        S_bf2 = [T(s1, "S_bf%d" % i, [128, 256], BF16) for i in range(2)]
        sq = T(s1, "sq", [128, 512], F32)
        hst = T(s1, "hst", [128, 12], F32)
        on_bf = T(s1, "on_bf", [128, 512], BF16)
        onT = T(s1, "onT", [128, 4, 128], BF16)
        pm = T(s1, "pm", [128, 4, 128], BF16)
        mix = T(s1, "mix", [128, 4, 128], BF16)
        mrg = T(s1, "mrg", [128, D], BF16)
        mT = T(s1, "mT", [128, 8, 128], BF16)
        x2s = T(s1, "x2s", [128, D], F32)
        h2b = T(s1, "h2b", [128, D], BF16)
        h2T = T(s1, "h2T", [128, 8, 128], BF16)

        pT = T(s1, "pT", [128, D], BF16, psum=True)
        ring = [T(s1, "ring%d" % i, [128, 512], F32, psum=True) for i in range(3)]
        CZ = T(s1, "CZ", [128, 512], F32, psum=True)
        b_cza, b_czb = Buf("cza"), Buf("czb")
        CR = T(s1, "CR", [128, 512], F32, psum=True)
        CS = T(s1, "CS", [128, 512], F32, psum=True)
        CO = T(s1, "CO", [128, 512], F32, psum=True)
        CS3 = CS.t[:, :].rearrange("p (h t) -> p h t", h=4)
        rstate = [0]

        def ring_next():
            r = ring[rstate[0] % 3]
            rstate[0] += 1
            return r

        stg_ring = [sgg[0], sgp[0], sgg[1], sgp[1]]
        p.dma("act", DMA(g2b.t[:], g2_d.partition_broadcast(128)), writes=[g2b.b], sem="c5")
        sstate = [0]

        def stg_next():
            r = stg_ring[sstate[0] % len(stg_ring)]
            sstate[0] += 1
            return r
        ceng = ["dve", "pool"]
        w_in_v = w_in_d.rearrange("(k p) n -> p k n", p=128)
        for cbk in range(33):
            c0 = cbk * 128
            wdt = 128 if cbk < 32 else 16
            sg_ = stg_next()
            st3 = sg_.t[:, :].rearrange("p (k n) -> p k n", k=8)
            p.dma("sp", DMA(st3[:, :, 0:wdt], w_in_v[:, :, c0:c0 + wdt]), writes=[sg_.b], sem="stg" + sg_.b.name)
            p.op("dve", TT(w_in.t[:, :, c0:c0 + wdt], st3[:, :, 0:wdt],
                                   g1col.unsqueeze(2).to_broadcast([128, 8, wdt]), ALU.mult),
                 reads=[sg_.b, cols.b], writes=[w_in.b])
        wgb_v = wgb_d.rearrange("(k p) n -> p k n", p=128)
        wpb_v = wpb_d.rearrange("(k p) n -> p k n", p=128)
        wout_v = wout_d.rearrange("(k p) n -> p k n", p=128)
        for k in range(4):
            sg_ = stg_next()
            p.dma("sp", DMA(sg_.t[:, :], wgb_v[:, k, :]), writes=[sg_.b], sem="stg" + sg_.b.name)
            p.op("act", ACT(wgb.t[:, k, :], sg_.t[:, :], AF.Copy, scale=glncol[:, k:k + 1]),
                 reads=[sg_.b, cols.b], writes=[wgb.b])
            sg_ = stg_next()
            p.dma("sp", DMA(sg_.t[:, :], wpb_v[:, k, :]), writes=[sg_.b], sem="stg" + sg_.b.name)
            p.op("act", ACT(wpb.t[:, k, :], sg_.t[:, :], AF.Copy, scale=psccol[:, k:k + 1]),
                 reads=[sg_.b, cols.b], writes=[wpb.b])
        for k in range(8):
            sg_ = stg_next()
            p.dma("sp", DMA(sg_.t[:, :], wout_v[:, k, :]), writes=[sg_.b], sem="stg" + sg_.b.name)
            if k % 2 == 0:
                p.op("dve", CP(wout.t[:, k, :], sg_.t[:, :]), reads=[sg_.b], writes=[wout.b])
            else:
                p.op("act", ACP(wout.t[:, k, :], sg_.t[:, :]), reads=[sg_.b], writes=[wout.b])
        sg_ = stg_next()
        stp = sg_.t[:, 0:512].rearrange("p (g d) -> p g d", g=4)
        p.dma("sp", DMA(stp, poolw_d.rearrange("g c d -> c g d")), writes=[sg_.b], sem="stg" + sg_.b.name)
        p.op("dve", CP(poolw.t[:], stp), reads=[sg_.b], writes=[poolw.b])
        sg_ = stg_next()
        str_ = sg_.t[:, 0:288].rearrange("p (k n) -> p k n", k=8)
        p.dma("sp", DMA(str_, wr_d.rearrange("(k p) n -> p k n", p=128)), writes=[sg_.b], sem="stg" + sg_.b.name)
        p.op("dve", CP(wr.t[:], str_), reads=[sg_.b], writes=[wr.b])
        sg_ = stg_next()
        p.dma("sp", DMA(sg_.t[0:1, 0:36], br_d), writes=[sg_.b], sem="stg" + sg_.b.name)
        p.op("dve", CP(brb.t[:], sg_.t[0:1, 0:36]), reads=[sg_.b], writes=[brb.b])
        p.dma("act", DMA(wa.t[:], wa_d), writes=[wa.b], sem="c4")
        for z_ in zl:
            p.op("pool", MSET(z_.t[:], 1.0), writes=[z_.b])
        p.op("pool", MSET(qz.t[:], 0.0), writes=[qz.b])

        def convert_dma(e, pc):
            if pc < 2:
                src = (weg_d if pc == 0 else weu_d)[e].rearrange("(k p) n -> p k n", p=128)
                dst = wall_t[e][:, 0:4096].rearrange("p (k n) -> p k n", k=8)[:, :, pc * 256:(pc + 1) * 256]
                p.dma("pool", DMA(dst, src), writes=[b_wgu[e]], sem="cv%d" % (pc))
            else:
                src = wed_d[e].rearrange("(c p) n -> p c n", p=128)
                dst = wall_t[e][:, 4096:6144].rearrange("p (c n) -> p c n", c=2)
                p.dma("pool", DMA(dst, src), writes=[b_wd[e]], sem="cv2")

        def gt(n):
            return (n % 2) * TPS + n // 2

        def xload(t):
            Xn = xs[t % 4]
            p.dma("sp", DMA(Xn.t[:], x_t[gt(t)]), writes=[Xn.b], sem="xl%d" % (t % 4))

        def proj_tok(t, R, c0, n, bufs):
            for kc in range(8):
                p.op("pe", MM(R.t[:, 0:n], hT.t[:, kc, :], w_in.t[:, kc, c0:c0 + n], kc == 0, kc == 7),
                     reads=[w_in.b, hT.b], writes=bufs)

        def x_pre(t):
            X, st = xs[t % 4], stt_[t % 2]
            p.op("act", ACT(h_bf.t[:], X.t[:], AF.Square, accum_out=st.t[:, 0:1]), reads=[X.b], writes=[h_bf.b, st.b])
            p.op("act", ACT(st.t[:, 1:2], st.t[:, 0:1], AF.Ln, scale=1.0 / D, bias=EPS), reads=[st.b], writes=[st.b])
            p.op("act", ACT(st.t[:, 2:3], st.t[:, 1:2], AF.Exp, scale=-0.5), reads=[st.b], writes=[st.b])
            p.op("act", ACT(h_bf.t[:], X.t[:], AF.Copy, scale=st.t[:, 2:3]), reads=[X.b, st.b], writes=[h_bf.b])

        def x_hT(t):
            for kc in range(8):
                p.op("pe", TR(pT.t[:, kc * 128:(kc + 1) * 128], h_bf.t[:, kc * 128:(kc + 1) * 128], ident),
                     reads=[h_bf.b, cb.b], writes=[pT.b])
            p.op("dve", CP(hT.t[:].rearrange("p k t -> p (k t)"), pT.t[:]), reads=[pT.b], writes=[hT.b])

        def x_g1(t):
            R = ring_next()
            proj_tok(t, R, 0, 512, [R.b])
            p.op("act", ACP(qk_tok[t % 2].t[:], R.t[:, :]), reads=[R.b], writes=[qk_tok[t % 2].b])

        def x_g1b(t):
            Q = qk_tok[t % 2]
            for c in range(4):
                p.op("pe", TR(pT.t[:, c * 128:(c + 1) * 128], Q.t[:, c * 128:(c + 1) * 128], ident),
                     reads=[Q.b, cb.b], writes=[pT.b])
            p.op("dve", CP(qkT[t % 2].t[:].rearrange("p c t -> p (c t)"), pT.t[:, 0:512]), reads=[pT.b],
                 writes=[qkT[t % 2].b])

        def x_g2(t):
            R = ring_next()
            for kc in range(8):
                p.op("pe", MM(R.t[0:16, 0:128], w_in.t[:, kc, 1536:1552], hT.t[:, kc, :], kc == 0, kc == 7),
                     reads=[w_in.b, hT.b], writes=[R.b])
            p.op("dve", CP(zl[t % 2].t[0:16, :], R.t[0:16, 0:128]), reads=[R.b], writes=[zl[t % 2].b])

        def x_g3(t):
            R = ring_next()
            proj_tok(t, R, 512, 512, [R.b])
            p.op("act", ACP(v_bf[t % 2].t[:], R.t[:, :]), reads=[R.b], writes=[v_bf[t % 2].b])

        def x_g5(t):
            R = ring_next()
            proj_tok(t, R, 1552, 512, [R.b])
            p.op("act", ACP(u_bf[t % 4].t[:], R.t[:, :]), reads=[R.b], writes=[u_bf[t % 4].b])

        def sig_evac(dst_ap, dst_buf, R):
            p.op("act", ACT(dst_ap, R.t[:, :], AF.Exp, scale=-1.0), reads=[R.b], writes=[dst_buf])
            p.op("act", ACT(dst_ap, dst_ap, AF.Ln, bias=1.0), reads=[dst_buf], writes=[dst_buf])
            p.op("act", ACT(dst_ap, dst_ap, AF.Exp, scale=-1.0), reads=[dst_buf], writes=[dst_buf])

        def x_gr(t):
            par = t % 2
            R = ring_next()
            proj_tok(t, R, 1024, 512, [R.b])
            p.op("dve", CP(r_sb.t[:], R.t[:, :]), reads=[R.b], writes=[r_sb.b])
            p.op("act", ACT(silu_r[par].t[:], r_sb.t[:], AF.Exp, scale=-1.0), reads=[r_sb.b],
                 writes=[silu_r[par].b])
            p.op("act", ACT(silu_r[par].t[:], silu_r[par].t[:], AF.Ln, bias=1.0), reads=[silu_r[par].b],
                 writes=[silu_r[par].b])
            p.op("act", ACT(silu_r[par].t[:], silu_r[par].t[:], AF.Exp, scale=-1.0), reads=[silu_r[par].b],
                 writes=[silu_r[par].b])
            p.op("pool", TT(silu_r[par].t[:], r_sb.t[:], silu_r[par].t[:], ALU.mult), reads=[r_sb.b, silu_r[par].b],
                 writes=[silu_r[par].b])

        def x_gate(which, hh):
            def f(t):
                dst = (sgg if which == 0 else sgp)[t % 2]
                c0 = 2064 if which == 0 else 3088
                R = ring_next()
                proj_tok(t, R, c0 + hh * 512, 512, [R.b])
                sig_evac(dst.t[:, hh * 512:(hh + 1) * 512], dst.b, R)
            return f
        x_gg0, x_gg1, x_gp0, x_gp1 = x_gate(0, 0), x_gate(0, 1), x_gate(1, 0), x_gate(1, 1)

        def y0(t):
            par = t % 2
            p.op("pe", MM(CZ.t[:, 0:256], zl[par].t[0:17, :], wa.t[0:17, :]), reads=[zl[par].b, wa.b], writes=[b_cza])
            p.op("act", ACT(lsp.t[:], CZ.t[:, 0:256], AF.Exp, scale=-1.0), reads=[b_cza], writes=[lsp.b])
            p.op("act", ACT(lsp.t[:], lsp.t[:], AF.Ln, bias=1.0), reads=[lsp.b], writes=[lsp.b])

        def y1(t):
            par = t % 2
            for c in range(2):
                p.op("pe", MM(CZ.t[:, 256 + c * 128:256 + (c + 1) * 128], lsp.t[:, c * 128:(c + 1) * 128], tri_incl),
                     reads=[lsp.b, cf.b], writes=[b_czb])
            p.op("pe", MM(CR.t[:, 0:256], tri_rev, lsp.t[:, :]), reads=[lsp.b, cf.b], writes=[CR.b])
            p.op("act", ACT(epos.t[:], CZ.t[:, 256:512], AF.Exp, bias=float(np.log(0.125))), reads=[b_czb],
                 writes=[epos.b])
            p.op("act", ACT(eneg.t[:], CZ.t[:, 256:512], AF.Exp, scale=-1.0), reads=[b_czb], writes=[eneg.b])
            p.op("act", ACT(dec.t[:], CZ.t[:, 256:512].rearrange("p (c t) -> p c t", c=2)[:, :, 127], AF.Exp),
                 reads=[b_czb], writes=[dec.b])
            p.op("act", ACT(erev.t[:], CR.t[:, 0:256], AF.Exp), reads=[CR.b], writes=[erev.b])
            for half in range(2):
                rows = slice(half * 64, (half + 1) * 64)
                p.op("dve", TT(qz.t[rows, half:4:2, :], qkT[par].t[rows, 0:2, :],
                               epos.t[rows, :].rearrange("p (c t) -> p c t", c=2), ALU.mult),
                     reads=[qkT[par].b, epos.b], writes=[qz.b])
            p.op("dve", TT(kdT.t[:], qkT[par].t[:, 2:4, :], eneg.t[:].rearrange("p (c t) -> p c t", c=2), ALU.mult),
                 reads=[qkT[par].b, eneg.b], writes=[kdT.b])
            p.op("pool", TT(kend.t[:], qk_tok[par].t[:, 256:512], erev.t[:], ALU.mult),
                 reads=[qk_tok[par].b, erev.b], writes=[kend.b])

        def y2(t):
            for h in range(4):
                p.op("pe", MM(CS3[:, h, :], kdT.t[:, h // 2, :], qz.t[:, h, :]), reads=[kdT.b, qz.b], writes=[CS.b])
            p.op("dve", TT(scT.t[:].rearrange("p h t -> p (h t)"), CS.t[:, :], cmask, ALU.mult),
                 reads=[CS.b, cf.b], writes=[scT.b])

        def y3(t):
            par = t % 2
            V = v_bf[par]
            S, S_bf = S2[t % 2], S_bf2[t % 2]
            if t // 2 == 0:
                p.op("pool", MSET(S.t[:], 0.0), writes=[S.b])
                p.op("pool", MSET(S_bf.t[:], 0.0), writes=[S_bf.b])
            for h in range(4):
                pr = h // 2
                p.op("pe", MM(CO.t[:, h * 128:(h + 1) * 128], scT.t[:, h, :], V.t[:, h * 128:(h + 1) * 128],
                              True, False), reads=[scT.b, V.b], writes=[CO.b])
                p.op("pe", MM(CO.t[:, h * 128:(h + 1) * 128], qz.t[:, h, :],
                              S_bf.t[:, pr * 128:(pr + 1) * 128], False, True),
                     reads=[qz.b, S_bf.b], writes=[CO.b])
            for pr in range(2):
                p.op("pe", MM(CR.t[:, pr * 256:(pr + 1) * 256], kend.t[:, pr * 128:(pr + 1) * 128],
                              V.t[:, pr * 256:(pr + 1) * 256]), reads=[kend.b, V.b], writes=[CR.b])
            for pr in range(2):
                for half in range(2):
                    rows = slice(half * 64, (half + 1) * 64)
                    p.op("dve", STT(S.t[rows, pr * 128:(pr + 1) * 128], S.t[rows, pr * 128:(pr + 1) * 128],
                                    dec.t[rows, pr:pr + 1],
                                    CR.t[rows, pr * 256 + half * 128:pr * 256 + (half + 1) * 128],
                                    ALU.mult, ALU.add), reads=[S.b, dec.b, CR.b], writes=[S.b])
            p.op("pool", CP(S_bf.t[:], S.t[:]), reads=[S.b], writes=[S_bf.b])
            p.op("act", ACT(sq.t[:], CO.t[:, :], AF.Square), reads=[CO.b], writes=[sq.b])
            p.op("dve", RED(hst.t[:, 0:4], sq.t[:].rearrange("p (h t) -> p h t", h=4), ALU.add), reads=[sq.b],
                 writes=[hst.b])
            p.op("act", ACT(hst.t[:, 4:8], hst.t[:, 0:4], AF.Ln, scale=1.0 / 128, bias=EPS), reads=[hst.b],
                 writes=[hst.b])
            p.op("act", ACT(hst.t[:, 8:12], hst.t[:, 4:8], AF.Exp, scale=-0.5), reads=[hst.b], writes=[hst.b])
            p.op("dve", TT(sq.t[:].rearrange("p (h t) -> p h t", h=4),
                           CO.t[:, :].rearrange("p (h t) -> p h t", h=4),
                           hst.t[:, 8:12].unsqueeze(2).to_broadcast([128, 4, 128]), ALU.mult),
                 reads=[CO.b, hst.b], writes=[sq.b])
            p.op("pool", TT(on_bf.t[:], sq.t[:], silu_r[par].t[:], ALU.mult), reads=[sq.b, silu_r[par].b],
                 writes=[on_bf.b])

        def y4(t):
            for c in range(4):
                p.op("pe", TR(pT.t[:, c * 128:(c + 1) * 128], on_bf.t[:, c * 128:(c + 1) * 128], ident),
                     reads=[on_bf.b, cb.b], writes=[pT.b])
            p.op("dve", CP(onT.t[:].rearrange("p c t -> p (c t)"), pT.t[:, 0:512]), reads=[pT.b], writes=[onT.b])

        def y5(t):
            G = sgg[t % 2]
            for hh in range(2):
                R = ring_next()
                for c in range(4):
                    p.op("pe", MM(R.t[:, :], onT.t[:, c, :], wgb.t[:, c, hh * 512:(hh + 1) * 512], c == 0, c == 3),
                         reads=[onT.b, wgb.b], writes=[R.b])
                p.op("dve", TT(G.t[:, hh * 512:(hh + 1) * 512], G.t[:, hh * 512:(hh + 1) * 512], R.t[:, :], ALU.mult),
                     reads=[G.b, R.b], writes=[G.b])

        def y6(t):
            j = t // 2
            ucur, uprev = u_bf[t % 4], u_bf[(t - 2) % 4]
            Am = poolA1 if j == 0 else poolA
            for g in range(4):
                p.op("pe", MM(CS3[:, g, :], ucur.t[:, g * 128:(g + 1) * 128], Am[:, g, :], True, j == 0),
                     reads=[ucur.b, cb.b], writes=[CS.b])
                if j > 0:
                    p.op("pe", MM(CS3[:, g, :], uprev.t[:, g * 128:(g + 1) * 128], poolB[:, g, :], False, True),
                         reads=[uprev.b, cb.b], writes=[CS.b])
            p.op("act", ACP(pm.t[:].rearrange("p g t -> p (g t)"), CS.t[:, :]), reads=[CS.b], writes=[pm.b])

        def y7(t):
            for g in range(4):
                p.op("pe", MM(CO.t[:, g * 128:(g + 1) * 128], poolw.t[:, g, :], pm.t[:, g, :]),
                     reads=[poolw.b, pm.b], writes=[CO.b])
            p.op("dve", CP(mix.t[:].rearrange("p g t -> p (g t)"), CO.t[:, :]), reads=[CO.b], writes=[mix.b])

        def y8(t):
            G, Pg = sgg[t % 2], sgp[t % 2]
            for hh in range(2):
                R = ring_next()
                for g in range(4):
                    p.op("pe", MM(R.t[:, :], mix.t[:, g, :], wpb.t[:, g, hh * 512:(hh + 1) * 512], g == 0, g == 3),
                         reads=[mix.b, wpb.b], writes=[R.b])
                p.op("dve", TT(Pg.t[:, hh * 512:(hh + 1) * 512], Pg.t[:, hh * 512:(hh + 1) * 512], R.t[:, :],
                               ALU.mult), reads=[Pg.b, R.b], writes=[Pg.b])
            p.op("pool", TT(mrg.t[:], G.t[:], Pg.t[:], ALU.add), reads=[G.b, Pg.b], writes=[mrg.b])

        def y9(t):
            for kc in range(8):
                p.op("pe", TR(pT.t[:, kc * 128:(kc + 1) * 128], mrg.t[:, kc * 128:(kc + 1) * 128], ident),
                     reads=[mrg.b, cb.b], writes=[pT.b])
            p.op("dve", CP(mT.t[:].rearrange("p k t -> p (k t)"), pT.t[:]), reads=[pT.b], writes=[mT.b])

        def y10(t):
            X = xs[t % 4]
            for hh in range(2):
                R = ring_next()
                for kc in range(8):
                    p.op("pe", MM(R.t[:, :], mT.t[:, kc, :], wout.t[:, kc, hh * 512:(hh + 1) * 512],
                                  kc == 0, kc == 7), reads=[mT.b, wout.b], writes=[R.b])
                p.op("dve", TT(x2s.t[:, hh * 512:(hh + 1) * 512], R.t[:, :], X.t[:, hh * 512:(hh + 1) * 512],
                               ALU.add), reads=[R.b, X.b], writes=[x2s.b])
            if stop == "p1":
                p.dma("sp", DMA(out_t[gt(t)], x2s.t[:]), reads=[x2s.b], writes=[b_out[gt(t)]], sem="x2o")
            else:
                p.dma("sp", DMA(x2_t[gt(t)], x2s.t[:]), reads=[x2s.b], writes=[b_x2[gt(t)]], sem="x2o")
            if t + 4 < ntr:
                xload(t + 4)

        def y_norm2(t):
            st = stt_[t % 2]
            p.op("act", ACT(h2b.t[:], x2s.t[:], AF.Square, accum_out=st.t[:, 3:4]), reads=[x2s.b],
                 writes=[h2b.b, st.b])
            p.op("act", ACT(st.t[:, 4:5], st.t[:, 3:4], AF.Ln, scale=1.0 / D, bias=EPS), reads=[st.b], writes=[st.b])
            p.op("act", ACT(st.t[:, 5:6], st.t[:, 4:5], AF.Exp, scale=-0.5), reads=[st.b], writes=[st.b])
            p.op("dve", STT(h2b.t[:], x2s.t[:], st.t[:, 5:6], g2b.t[:], ALU.mult, ALU.mult),
                 reads=[x2s.b, st.b, g2b.b], writes=[h2b.b])
            p.dma("sp", DMA(h2_t[gt(t)], h2b.t[:]), reads=[h2b.b], writes=[b_h2[gt(t)]], sem="h2o")

        def y11(t):
            for kc in range(8):
                p.op("pe", TR(pT.t[:, kc * 128:(kc + 1) * 128], h2b.t[:, kc * 128:(kc + 1) * 128], ident),
                     reads=[h2b.b, cb.b], writes=[pT.b])
            p.op("dve", CP(h2T.t[:].rearrange("p k t -> p (k t)"), pT.t[:]), reads=[pT.b], writes=[h2T.b])

        def y12(t):
            i = gt(t)
            RL = ring_next()
            for kc in range(8):
                p.op("pe", MM(RL.t[:, 0:36], h2T.t[:, kc, :], wr.t[:, kc, :], kc == 0, False),
                     reads=[h2T.b, wr.b], writes=[RL.b])
            p.op("pe", MM(RL.t[:, 0:36], ones_bf[0:1, :], brb.t[0:1, :], False, True), reads=[cb.b, brb.b],
                 writes=[RL.b])
            p.op("dve", CP(LG.t[:, i, :], RL.t[:, 0:36]), reads=[RL.b], writes=[LG.b])

        XA = (x_pre, x_hT, x_g1, x_g2, x_g1b, x_g3, x_g5)
        XB = (x_gr, x_gg0, x_gg1, x_gp0, x_gp1)
        Y1F = (y0, y1, y2, y3, y6, y7)
        for t_ in range(min(4, ntr)):
            xload(t_)
        for f_ in XA + XB:
            f_(0)
        if ntr > 1:
            for k_, f_ in enumerate(XA):
                f_(1)
                if k_ < len(Y1F):
                    Y1F[k_](0)
        else:
            for f_ in Y1F:
                f_(0)
        nconv = 0
        for m in range(ntr):
            hasXa = m + 2 < ntr
            hasXb = m + 1 < ntr
            hasY1 = m + 1 < ntr

            def XA_(f_):
                if hasXa:
                    f_(m + 2)

            def XB_(f_):
                if hasXb:
                    f_(m + 1)

            def Y1(f_):
                if hasY1:
                    f_(m + 1)
            XA_(x_pre)
            y4(m)
            Y1(y0)
            XB_(x_gr)
            y5(m)
            Y1(y1)
            y8(m)
            XB_(x_gg0)
            Y1(y6)
            XB_(x_gg1)
            Y1(y2)
            y9(m)
            Y1(y7)
            XB_(x_gp0)
            Y1(y3)
            XB_(x_gp1)
            XA_(x_hT)
            y10(m)
            y_norm2(m)
            XA_(x_g1)
            y11(m)
            XA_(x_g2)
            y12(m)
            XA_(x_g3)
            XA_(x_g1b)
            XA_(x_g5)
            if stop != "p1" and m < NXZ:
                p.dma("pool", DMA(xs_flat[:, m * 8192:(m + 1) * 8192], zeros_d), writes=[b_xz[m]], sem="xz%d" % m)
            if stop != "p1":
                for _ in range(2):
                    if nconv < NE * 3:
                        convert_dma(nconv // 3, nconv % 3)
                        nconv += 1
        if stop == "p1":
            p.barrier()
            s1.close()
            p.emit(top)
            return nc
        assert nconv == NE * 3

        p.barrier()
        s1.close()

        s15 = contextlib.ExitStack()
        Msum = T(s15, "Msum", [128, NT * 32], BF16)
        cntS = T(s15, "cntS", [128, NT, 32], F32)
        preS = T(s15, "preS", [128, NT, 32], F32)
        baseS = T(s15, "baseS", [128, NT, 32], F32)
        sm = T(s15, "sm", [128, 8, 32], F32)
        cmp1 = T(s15, "cmp1", [128, 32, 32], F32)
        cmp2 = T(s15, "cmp2", [128, NWB, 32], F32)
        destf = T(s15, "destf", [128, 2, NT], F32)
        bef = T(s15, "bef", [128, NWB], F32)
        h2r = [T(s15, "h2r%d" % i, [128, D], BF16) for i in range(4)]
        cps = T(s15, "cps", [128, NT * 32], F32, psum=True)
        pps = T(s15, "pps", [128, NT * 32], F32, psum=True)

        Mall = T(s15, "Mall", [128, 2, NT, 32], BF16)
        rg = T(s15, "rg", [128, 8, NT, 4], F32)
        re_ = T(s15, "re", [128, 4, NT, 8], F32)
        rs = T(s15, "rs", [128, 8, NT], F32)
        rbig = T(s15, "rbig", [128, NT, 32], F32)
        gl = LG.t[:, :, 0:4]
        el = LG.t[:, :, 4:36].rearrange("p n (g j) -> p n g j", g=4)
        gsub, goh = rg.t[:, 0], rg.t[:, 1]
        esel, oh1, esel2, oh2 = (re_.t[:, k_] for k_ in range(4))
        gmax, gsum, m1, m2, dd, e21, den = (rs.t[:, k_] for k_ in range(7))
        RB = [rg.b, re_.b, rs.b]
        p.op("dve", RED(gmax, gl, ALU.max), reads=[LG.b], writes=RB)
        p.op("dve", TT(gsub, gl, gmax.unsqueeze(2).to_broadcast([128, NT, 4]), ALU.subtract), reads=[LG.b] + RB,
             writes=RB)
        p.op("act", ACT(gsub, gsub, AF.Exp), reads=RB, writes=RB)
        p.op("dve", RED(gsum, gsub, ALU.add), reads=RB, writes=RB)
        p.op("dve", TT(goh, gl, gmax.unsqueeze(2).to_broadcast([128, NT, 4]), ALU.is_equal), reads=[LG.b] + RB,
             writes=RB)
        p.op("dve", TT(rbig.t[:].rearrange("p n (g j) -> p n g j", g=4), el,
                       goh.unsqueeze(3).to_broadcast([128, NT, 4, 8]), ALU.mult), reads=[LG.b] + RB, writes=[rbig.b])
        p.op("dve", RED(esel, rbig.t[:].rearrange("p n (g j) -> p n j g", g=4), ALU.add), reads=[rbig.b], writes=RB)
        p.op("dve", RED(m1, esel, ALU.max), reads=RB, writes=RB)
        p.op("dve", TT(oh1, esel, m1.unsqueeze(2).to_broadcast([128, NT, 8]), ALU.is_equal), reads=RB, writes=RB)
        p.op("dve", STT(esel2, oh1, -1e30, esel, ALU.mult, ALU.add), reads=RB, writes=RB)
        p.op("dve", RED(m2, esel2, ALU.max), reads=RB, writes=RB)
        p.op("dve", TT(oh2, esel2, m2.unsqueeze(2).to_broadcast([128, NT, 8]), ALU.is_equal), reads=RB, writes=RB)
        p.op("dve", TT(dd, m2, m1, ALU.subtract), reads=RB, writes=RB)
        p.op("act", ACT(e21, dd, AF.Exp), reads=RB, writes=RB)
        p.op("dve", STT(den, e21, 1.0, gsum, ALU.add, ALU.mult), reads=RB, writes=RB)
        p.op("dve", RCP(Wall.t[:, :, 0], den), reads=RB, writes=[Wall.b])
        p.op("dve", TT(Wall.t[:, :, 1], Wall.t[:, :, 0], e21, ALU.mult), reads=RB + [Wall.b], writes=[Wall.b])
        for k_, ohk in ((0, oh1), (1, oh2)):
            p.op("dve", TT(Mall.t[:, k_].rearrange("p n (g j) -> p n g j", g=4),
                           goh.unsqueeze(3).to_broadcast([128, NT, 4, 8]),
                           ohk.unsqueeze(2).to_broadcast([128, NT, 4, 8]), ALU.mult), reads=RB, writes=[Mall.b])
        p.op("pool", TT(Msum.t[:], Mall.t[:, 0].rearrange("p n e -> p (n e)"),
                        Mall.t[:, 1].rearrange("p n e -> p (n e)"), ALU.add), reads=[Mall.b], writes=[Msum.b])
        for q in range(4):
            p.op("pe", MM(cps.t[:, q * 512:(q + 1) * 512], ones_bf, Msum.t[:, q * 512:(q + 1) * 512]),
                 reads=[cb.b, Msum.b], writes=[cps.b])
            p.op("pe", MM(pps.t[:, q * 512:(q + 1) * 512], lstrict, Msum.t[:, q * 512:(q + 1) * 512]),
                 reads=[cb.b, Msum.b], writes=[pps.b])
        p.op("act", ACP(cntS.t[:].rearrange("p n e -> p (n e)"), cps.t[:]), reads=[cps.b], writes=[cntS.b])
        p.op("dve", CP(preS.t[:].rearrange("p n e -> p (n e)"), pps.t[:]), reads=[pps.b], writes=[preS.b])
        p.op("dve", MSET(baseS.t[:, 0, :], 0.0), writes=[baseS.b])
        for i in range(1, NT):
            p.op("dve", TT(baseS.t[:, i, :], baseS.t[:, i - 1, :], cntS.t[:, i - 1, :], ALU.add),
                 reads=[baseS.b, cntS.b], writes=[baseS.b])
        tot, nblk, padded, pend, pstart, ones32 = (sm.t[:, k, :] for k in range(6))
        smb = [sm.b]
        p.op("dve", TT(tot, baseS.t[:, NT - 1, :], cntS.t[:, NT - 1, :], ALU.add), reads=[baseS.b, cntS.b], writes=smb)
        p.op("dve", TT(cmp1.t[:], tot.unsqueeze(2).to_broadcast([128, 32, 32]),
                       thr[:, 0:64:2].unsqueeze(1).to_broadcast([128, 32, 32]), ALU.is_gt), reads=smb + [cf.b],
             writes=[cmp1.b])
        p.op("dve", RED(nblk, cmp1.t[:], ALU.add), reads=[cmp1.b], writes=smb)
        p.op("dve", TS(padded, nblk, 256.0, ALU.mult), reads=smb, writes=smb)
        p.op("dve", MSET(ones32, 1.0), writes=smb)
        p.op("dve", lambda e: e.tensor_tensor_scan(out=pend, data0=ones32, data1=padded, initial=0.0,
                                                   op0=ALU.mult, op1=ALU.add), reads=smb, writes=smb)
        p.op("dve", TT(pstart, pend, padded, ALU.subtract), reads=smb, writes=smb)
        p.op("dve", TT(preS.t[:], preS.t[:], baseS.t[:], ALU.add), reads=[preS.b, baseS.b], writes=[preS.b])
        p.op("dve", TT(preS.t[:], preS.t[:], pstart.unsqueeze(1).to_broadcast([128, NT, 32]), ALU.add),
             reads=[preS.b] + smb, writes=[preS.b])
        for k in range(2):
            p.op("dve", TT(baseS.t[:], Mall.t[:, k], preS.t[:], ALU.mult), reads=[Mall.b, preS.b], writes=[baseS.b])
            p.op("dve", RED(destf.t[:, k, :], baseS.t[:], ALU.add), reads=[baseS.b], writes=[destf.b])
        p.op("dve", CP(desti.t[:], destf.t[:]), reads=[destf.b], writes=[desti.b])
        p.op("dve", TT(cmp2.t[:], pend.unsqueeze(1).to_broadcast([128, NWB, 32]),
                       bvals.unsqueeze(2).to_broadcast([128, NWB, 32]), ALU.is_le), reads=smb + [cf.b],
             writes=[cmp2.b])
        p.op("dve", RED(bef.t[:], cmp2.t[:], ALU.add), reads=[cmp2.b], writes=[bef.b])
        p.op("dve", TS(bef.t[:], bef.t[:], 31.0, ALU.min), reads=[bef.b], writes=[bef.b])
        p.op("dve", TS(bef.t[:], bef.t[:], 128.0, ALU.mult, pidx, ALU.add), reads=[bef.b, cf.b], writes=[bef.b])
        p.op("dve", CP(widxi.t[:], bef.t[:]), reads=[bef.b], writes=[widxi.b])
        def h2load(i):
            Hr = h2r[i % 4]
            p.dma("sp", DMA(Hr.t[:], h2_t[i]), reads=[b_h2[i]], writes=[Hr.b], sem="h2l%d" % (i % 4))
        for i in range(3):
            h2load(i)
        for i in range(NT):
            Hr = h2r[i % 4]
            if i + 3 < NT:
                h2load(i + 3)
            for k in range(2):
                p.dma("pool", SCAT(xs_d, desti.t[:, k, i:i + 1], Hr.t[:]), reads=[Hr.b, desti.b] + b_xz,
                      writes=[b_sc[2 * i + k]], sem="sc%d%d" % (i % 4, k))
        p.barrier()
        s15.close()

        s2 = contextlib.ExitStack()
        NBX, NBW = 5, 3
        NBX2 = 3
        xb = [T(s2, "xb%d" % i, [128, 2, D], BF16) for i in range(NBX2)]
        xs_blk = xs_d.rearrange("(n h p) d -> n p h d", h=2, p=128)
        y_blk = y_d.rearrange("(n h p) d -> n p h d", h=2, p=128)
        wall = [T(s2, "walls%d" % i, [128, 6144], BF16) for i in range(NBW)]
        xbT = [T(s2, "xbT%d" % i, [128, 8, 128], BF16) for i in range(2)]
        sgm = [T(s2, "sgm%d" % i, [128, 256], F32) for i in range(2)]
        a_bf = [T(s2, "a_bf%d" % i, [128, 256], BF16) for i in range(2)]
        aT = [T(s2, "aT%d" % i, [128, 2, 128], BF16) for i in range(2)]
        ysb = [T(s2, "ysb%d" % i, [128, 2, D], F32) for i in range(2)]
        pTa = T(s2, "pTa", [128, D], BF16, psum=True)
        pTc = T(s2, "pTc", [128, 256], BF16, psum=True)
        pH = [T(s2, "pH%d" % i, [128, 512], F32, psum=True) for i in range(2)]
        pY = [T(s2, "pY%d" % i, [128, D], F32, psum=True) for i in range(2)]

        def xb_load(wb):
            r = wb % NBX2
            p.dma("sp", DMA(xb[r].t[:], xs_blk[wb]), reads=b_sc, writes=[xb[r].b], sem="xbl%d" % r)

        def w_load(wb):
            r = wb % NBW
            p.dma("pool", GATH(wall[r].t[:], wall_d, widxi.t[:, wb:wb + 1]), reads=b_wgu + b_wd + [widxi.b],
                  writes=[wall[r].b], sem="wgl%d" % r)

        def stA(b):
            q, X_ = b % 2, xb[(b // 2) % NBX2]
            for kc in range(8):
                p.op("pe", TR(pTa.t[:, kc * 128:(kc + 1) * 128], X_.t[:, b % 2, kc * 128:(kc + 1) * 128], ident),
                     reads=[X_.b, cb.b], writes=[pTa.b])
            p.op("dve", CP(xbT[q].t[:].rearrange("p k t -> p (k t)"), pTa.t[:]), reads=[pTa.b], writes=[xbT[q].b])

        def stB(b):
            q, W_ = b % 2, wall[(b // 2) % NBW]
            w3 = W_.t[:, 0:4096].rearrange("p (k n) -> p k n", k=8)
            for kc in range(8):
                p.op("pe", MM(pH[q].t[:, :], xbT[q].t[:, kc, :], w3[:, kc, :], kc == 0, kc == 7),
                     reads=[xbT[q].b, W_.b], writes=[pH[q].b])
            p.op("act", ACT(sgm[q].t[:], pH[q].t[:, 0:256], AF.Sigmoid), reads=[pH[q].b], writes=[sgm[q].b])
            p.op("dve", TT(sgm[q].t[:], sgm[q].t[:], pH[q].t[:, 0:256], ALU.mult), reads=[sgm[q].b, pH[q].b],
                 writes=[sgm[q].b])
            p.op("dve", TT(a_bf[q].t[:], sgm[q].t[:], pH[q].t[:, 256:512], ALU.mult), reads=[sgm[q].b, pH[q].b],
                 writes=[a_bf[q].b])

        def stC(b):
            q = b % 2
            for c in range(2):
                p.op("pe", TR(pTc.t[:, c * 128:(c + 1) * 128], a_bf[q].t[:, c * 128:(c + 1) * 128], ident),
                     reads=[a_bf[q].b, cb.b], writes=[pTc.b])
            p.op("act", ACP(aT[q].t[:].rearrange("p c t -> p (c t)"), pTc.t[:, :]), reads=[pTc.b], writes=[aT[q].b])

        def stD(b):
            q, W_ = b % 2, wall[(b // 2) % NBW]
            d3 = W_.t[:, 4096:6144].rearrange("p (c n) -> p c n", c=2)
            for hh in range(2):
                for c in range(2):
                    p.op("pe", MM(pY[q].t[:, hh * 512:(hh + 1) * 512], aT[q].t[:, c, :],
                                  d3[:, c, hh * 512:(hh + 1) * 512], c == 0, c == 1),
                         reads=[aT[q].b, W_.b], writes=[pY[q].b])
            Yb = ysb[(b // 2) % 2]
            p.op("act", ACP(Yb.t[:, b % 2, 0:512], pY[q].t[:, 0:512]), reads=[pY[q].b], writes=[Yb.b])
            p.op("dve", CP(Yb.t[:, b % 2, 512:1024], pY[q].t[:, 512:1024]), reads=[pY[q].b], writes=[Yb.b])
            if b % 2 == 1:
                p.dma("sp", DMA(y_blk[b // 2], Yb.t[:]), reads=[Yb.b], writes=[b_y[b - 1], b_y[b]],
                      sem="yo%d" % ((b // 2) % 2))

        for b in range(3):
            xb_load(b)
        for b in range(2):
            w_load(b)
        stA(0)
        stA(1)
        stB(0)
        for i in range(NBLK):
            if i % 2 == 0 and i // 2 + 3 < NWB:
                xb_load(i // 2 + 3)
            if i % 2 == 0 and i // 2 + 2 < NWB:
                w_load(i // 2 + 2)
            if i + 2 < NBLK:
                stA(i + 2)
            stC(i)
            if i + 1 < NBLK:
                stB(i + 1)
            stD(i)
        p.barrier()
        s2.close()

        s3 = contextlib.ExitStack()
        NB3 = 3
        NP3 = NT // 2
        xa = [T(s3, "xa%d" % i, [128, 2, D], F32) for i in range(NB3)]
        y0 = [T(s3, "y0%d" % i, [128, D], F32) for i in range(4)]
        y1 = [T(s3, "y1%d" % i, [128, D], F32) for i in range(4)]
        ob = [T(s3, "ob%d" % i, [128, 2, D], F32) for i in range(2)]
        junk3 = T(s3, "junk3", [128, D], BF16)
        st3_ = [T(s3, "st3%d" % i, [128, 4], F32) for i in range(2)]
        x2_pair = x2_d.rearrange("(n h p) d -> n p h d", h=2, p=128)
        out_pair = out_d.rearrange("(n h p) d -> n p h d", h=2, p=128)

        def p3_xload(j):
            r = j % NB3
            p.dma("sp", DMA(xa[r].t[:], x2_pair[j]), reads=[b_x2[2 * j], b_x2[2 * j + 1]], writes=[xa[r].b],
                  sem="x2l%d" % r)

        def p3_gath(i):
            r = i % 4
            p.dma("pool", GATH(y0[r].t[:], y_d, desti.t[:, 0, i:i + 1]), reads=b_y + [desti.b], writes=[y0[r].b],
                  sem="y0l%d" % r)
            p.dma("pool", GATH(y1[r].t[:], y_d, desti.t[:, 1, i:i + 1]), reads=b_y + [desti.b], writes=[y1[r].b],
                  sem="y1l%d" % r)
        p3_xload(0)
        p3_xload(1)
        for i_ in range(3):
            p3_gath(i_)
        for i in range(NT):
            j, h = i // 2, i % 2
            if h == 0 and j + 2 < NP3:
                p3_xload(j + 2)
            if i + 3 < NT:
                p3_gath(i + 3)
            A = xa[j % NB3].t[:, h, :]
            Ab = xa[j % NB3].b
            Y0, Y1, stq = y0[i % 4], y1[i % 4], st3_[i % 2]
            Ob = ob[j % 2]
            O = Ob.t[:, h, :]
            p.op("dve", STT(A, Y0.t[:], Wall.t[:, i, 0:1], A, ALU.mult, ALU.add),
                 reads=[Y0.b, Wall.b, Ab], writes=[Ab])
            p.op("dve", STT(A, Y1.t[:], Wall.t[:, i, 1:2], A, ALU.mult, ALU.add),
                 reads=[Y1.b, Wall.b, Ab], writes=[Ab])
            p.op("act", ACT(junk3.t[:], A, AF.Square, accum_out=stq.t[:, 0:1]), reads=[Ab],
                 writes=[junk3.b, stq.b])
            p.op("act", ACT(stq.t[:, 1:2], stq.t[:, 0:1], AF.Ln, scale=1.0 / D, bias=EPS), reads=[stq.b],
                 writes=[stq.b])
            p.op("act", ACT(stq.t[:, 2:3], stq.t[:, 1:2], AF.Exp, scale=-0.5), reads=[stq.b], writes=[stq.b])
            p.op("dve", STT(O, A, stq.t[:, 2:3], gf.t[:], ALU.mult, ALU.mult),
                 reads=[Ab, stq.b, gf.b], writes=[Ob.b])
            if h == 1:
                p.dma("sp", DMA(out_pair[j], Ob.t[:]), reads=[Ob.b], writes=[b_out[i - 1], b_out[i]],
                      sem="oo%d" % (j % 2))
        p.wait_only("sp", reads=b_out)
        s3.close()
        p.emit(top)
    return nc


def _constants():
    s = np.arange(128)[:, None]
    t = np.arange(128)[None, :]
    cf = np.zeros((128, NCF), np.float32)
    cf[:, CF_TRI:CF_TRI + 128] = np.where(s <= t, -1.0 / 16.0, 0.0)
    cf[:, CF_REV:CF_REV + 128] = np.where(s > t, -1.0 / 16.0, 0.0)
    cf[:, CF_MASK:CF_MASK + 512] = np.tile(np.where(s <= t, 1.0, 0.0), (1, 4))
    cf[:, CF_THR:CF_THR + 64] = 128.0 * np.arange(64)[None, :]
    cf[:, CF_BV:CF_BV + NWB] = 256.0 * np.arange(NWB)[None, :]
    cf[:, CF_PIDX] = np.arange(128)
    cb = np.zeros((128, NCB), np.float32)
    cb[:, CB_ID:CB_ID + 128] = np.eye(128)
    cb[:, CB_ONES:CB_ONES + 128] = 1.0
    cb[:, CB_LS:CB_LS + 128] = np.where(s < t, 1.0, 0.0)
    for g, w in enumerate((2, 4, 8, 16)):
        A = np.where((s <= t) & (s > t - w), 1.0 / w, 0.0) - np.where(s == t, 1.0, 0.0)
        B = np.where(s - 128 > t - w, 1.0 / w, 0.0)
        cnt = np.minimum(t + 1, w).astype(np.float64)
        A1 = np.where((s <= t) & (s > t - w), 1.0 / cnt, 0.0) - np.where(s == t, 1.0, 0.0)
        cb[:, CB_A + g * 128:CB_A + (g + 1) * 128] = A
        cb[:, CB_B + g * 128:CB_B + (g + 1) * 128] = B
        cb[:, CB_A1 + g * 128:CB_A1 + (g + 1) * 128] = A1
    return cf, cb.astype(ml_dtypes.bfloat16)


_NC = None


def kernel(x, norm1_g, w_in, w_alpha_up, b_alpha, gla_norm_g, w_gla_branch, pool_w, pool_scale,
           w_pool_branch, w_out, norm2_g, w_router_group, b_router_group, w_router_expert,
           b_router_expert, w_exp_gate, w_exp_up, w_exp_down, norm_f_g):
    global _NC
    f = lambda a: np.ascontiguousarray(np.asarray(a), dtype=np.float32)
    x = f(x)
    cf, cb = _constants()
    cols = np.concatenate([
        f(norm1_g)[0].reshape(8, 128).T, f(norm2_g)[0].reshape(8, 128).T,
        f(gla_norm_g)[0].reshape(4, 128).T, f(pool_scale)[0].reshape(4, 128).T], axis=1)
    wr = np.concatenate([f(w_router_group)[0], f(w_router_expert)[0].transpose(1, 0, 2).reshape(D, 32)], axis=1)
    br = np.concatenate([f(b_router_group)[0], f(b_router_expert)[0].reshape(32)])[None, :]
    wa = np.concatenate([f(w_alpha_up)[0], f(b_alpha)[0][None, :]], axis=0)
    shared = dict(
        w_in=f(w_in)[0], wa=f(wa), cols=f(cols), wgb=f(w_gla_branch)[0], wpb=f(w_pool_branch)[0],
        wout=f(w_out)[0], poolw=f(pool_w)[0], wr=f(wr), br=f(br), weg=f(w_exp_gate)[0], weu=f(w_exp_up)[0],
        wed=f(w_exp_down)[0], gf=f(norm_f_g), g2=f(norm2_g)[0], cf=cf, cb=cb,
        zeros=np.zeros((128, 8192), ml_dtypes.bfloat16))
    if _NC is None:
        _NC = build_program()
    in_maps = []
    for c in range(NCORES):
        m = dict(shared)
        m["x"] = np.ascontiguousarray(x[2 * c:2 * c + 2].reshape(TOK, D))
        in_maps.append(m)
    res = run_bass_kernel_spmd(_NC, in_maps, core_ids=list(range(NCORES)))
    out = np.concatenate([np.asarray(r["out"]).reshape(2, SEQ, D) for r in res.results], axis=0)
    return out.astype(np.float32)
```
